# Optimizing a Trainium2 kernel written in Bass

```python
import jax
import jax.numpy as jnp
from jax import lax
import numpy as np


D_MODEL = 1024
BATCH = 8
SEQ = 2048
DEPTH = 4

N_BRANCH = 4
BRANCH_WIDTH = 512
RWKV_HEADS = 8
RWKV_HEAD_DIM = BRANCH_WIDTH // RWKV_HEADS
RWKV_DECAY_RANK = 64
RWKV_ICL_RANK = 64
RWKV_VALUE_RANK = 32
RWKV_GATE_RANK = 128
RWKV_LNX_EPS = 64e-5
HGRN_HEADS = 4
HGRN_HEAD_DIM = BRANCH_WIDTH // HGRN_HEADS
MLSTM_HEADS = 4
MLSTM_HEAD_DIM = BRANCH_WIDTH // MLSTM_HEADS
CHUNK = 64
LRU_BLOCKS = 8
LRU_BLOCK_DIM = BRANCH_WIDTH // LRU_BLOCKS
LRU_C = 8.0
CONV_WIDTH = 4
N_EXPERTS = 32
TOP_K = 4
D_EXPERT = 1024
SWIGLU_LIMIT = 7.0
SWIGLU_ALPHA = 1.702
EXPERT_BLOCK = 128
D_PLE = 256
LN_EPS = 1e-5
NORM_EPS = 1e-6
DEEPNORM_ALPHA = (2 * DEPTH) ** 0.25
DEEPNORM_BETA = (8 * DEPTH) ** -0.25

RWKV_SPLITS = (BRANCH_WIDTH, BRANCH_WIDTH, BRANCH_WIDTH, RWKV_DECAY_RANK, RWKV_ICL_RANK, RWKV_GATE_RANK)
RWKV_COLS = 3 * BRANCH_WIDTH + RWKV_DECAY_RANK + RWKV_ICL_RANK + RWKV_GATE_RANK
HGRN_COLS = 4 * BRANCH_WIDTH
MLSTM_SPLITS = (BRANCH_WIDTH, BRANCH_WIDTH, BRANCH_WIDTH, BRANCH_WIDTH, MLSTM_HEADS, MLSTM_HEADS)
MLSTM_COLS = 4 * BRANCH_WIDTH + 2 * MLSTM_HEADS
LRU_COLS = 2 * BRANCH_WIDTH
GATE_COLS = N_BRANCH * D_MODEL
N_COLS_BASE = RWKV_COLS + HGRN_COLS + MLSTM_COLS + LRU_COLS + GATE_COLS
N_COLS_REST = N_COLS_BASE + RWKV_VALUE_RANK

F32 = jnp.float32

kernel_name = 'hybrid_rwkv7_hgrn2_mlstm_rglru_moe_deepnorm'


def _split(z, sizes):
    return jnp.split(z, np.cumsum(sizes)[:-1].tolist(), axis=-1)


def _layernorm(x, g, b, dtype):
    xf = x.astype(F32)
    xc = xf - jnp.mean(xf, -1, keepdims=True)
    var = jnp.mean(xc * xc, -1, keepdims=True)
    return (xc * lax.rsqrt(var + LN_EPS) * g + b).astype(dtype)


def _head_layernorm(y, eps):
    yc = y - jnp.mean(y, -1, keepdims=True)
    out = yc * lax.rsqrt(jnp.mean(yc * yc, -1, keepdims=True) + eps)
    return out.reshape(out.shape[:-2] + (-1,))


def _head_rmsnorm(y, eps):
    out = y * lax.rsqrt(jnp.mean(y * y, -1, keepdims=True) + eps)
    return out.reshape(out.shape[:-2] + (-1,))


def _token_shift(z):
    return jnp.pad(z, ((0, 0), (1, 0), (0, 0)))[:, :-1]


def _causal_conv(z, w, b):
    seq = z.shape[1]
    zp = jnp.pad(z, ((0, 0), (CONV_WIDTH - 1, 0), (0, 0)))
    out = b
    for j in range(CONV_WIDTH):
        out = out + zp[:, j:j + seq] * w[j]
    return out


def _to_heads(t, n_heads):
    bsz, seq, width = t.shape
    return t.reshape(bsz, seq, n_heads, width // n_heads).transpose(0, 2, 1, 3)


def _from_heads(t):
    return t.transpose(0, 2, 1, 3)


def _to_chunks(t):
    bsz, nh, seq = t.shape[:3]
    t = t.reshape((bsz, nh, seq // CHUNK, CHUNK) + t.shape[3:])
    return jnp.moveaxis(t, 2, 0)


def _from_chunks(t):
    nc, bsz, nh, c, dv = t.shape
    return jnp.moveaxis(t, 0, 2).reshape(bsz, nh, nc * c, dv)


def rwkv7_mix(zA, v_first, v_mix, mu, w0, w2, a0, a2, g2, k_k, k_a, r_k, lnx_g, lnx_b):
    bsz, seq = zA.shape[:2]
    zA = zA + (_token_shift(zA) - zA) * mu
    r, k, v, xw, xa, xg = _split(zA, RWKV_SPLITS)
    w_log = -jax.nn.softplus(-(w0 + jnp.tanh(xw) @ w2)) - 0.5
    decay = jnp.exp(-jnp.exp(w_log))
    a = jax.nn.sigmoid(a0 + xa @ a2)
    g = jax.nn.sigmoid(xg) @ g2
    if v_mix is not None:
        v = v + (v_first - v) * jax.nn.sigmoid(v_mix)

    def hs(t):
        return t.reshape(bsz, seq, RWKV_HEADS, RWKV_HEAD_DIM)

    kk = hs(k * k_k)
    kk = kk / jnp.maximum(jnp.sqrt(jnp.sum(kk * kk, -1, keepdims=True)), 1e-12)
    k = k * (1.0 + (a - 1.0) * k_a)
    rh, kh, vh, dh, ah = hs(r), hs(k), hs(v), hs(decay), hs(a)
    neg_kk = -kk
    kk_a = kk * ah

    def step(state, inp):
        r_t, w_t, k_t, v_t, a_t, b_t = inp
        sa = jnp.einsum('bhvk,bhk->bhv', state, a_t)
        state = state * w_t[:, :, None, :] + sa[..., None] * b_t[:, :, None, :] + v_t[..., None] * k_t[:, :, None, :]
        return state, jnp.einsum('bhvk,bhk->bhv', state, r_t)

    xs = tuple(jnp.moveaxis(t, 1, 0) for t in (rh, dh, kh, vh, neg_kk, kk_a))
    state0 = jnp.zeros((bsz, RWKV_HEADS, RWKV_HEAD_DIM, RWKV_HEAD_DIM), F32)
    _, y = lax.scan(step, state0, xs)
    y = jnp.moveaxis(y, 0, 1)
    y = _head_layernorm(y, RWKV_LNX_EPS) * lnx_g + lnx_b
    bonus = (jnp.sum(rh * kh * r_k, -1, keepdims=True) * vh).reshape(bsz, seq, BRANCH_WIDTH)
    return (y + bonus) * g, v


def _gla_chunked(q, k, v, log_f):
    bsz, nh, _, dk = q.shape
    dv = v.shape[-1]
    causal = jnp.tril(jnp.ones((CHUNK, CHUNK), bool))

    def chunk(state, inp):
        qc, kc, vc, lf = inp
        bcum = jnp.cumsum(lf, axis=-2)
        o_inter = jnp.einsum('bhtd,bhde->bhte', qc * jnp.exp(bcum), state)
        diff = bcum[:, :, :, None, :] - bcum[:, :, None, :, :]
        pair_decay = jnp.exp(jnp.where(causal[:, :, None], diff, -jnp.inf))
        attn = jnp.einsum('bhtd,bhsd,bhtsd->bhts', qc, kc, pair_decay)
        o = o_inter + jnp.einsum('bhts,bhse->bhte', attn, vc)
        b_last = bcum[:, :, -1]
        state = jnp.exp(b_last)[..., None] * state + jnp.einsum('bhsd,bhse->bhde', kc * jnp.exp(b_last[:, :, None] - bcum), vc)
        return state, o

    state0 = jnp.zeros((bsz, nh, dk, dv), F32)
    _, o = lax.scan(chunk, state0, tuple(_to_chunks(t) for t in (q, k, v, log_f)))
    return _from_chunks(o)


def hgrn2_mix(zB, lb, norm_w):
    q, f, i, g = _split(zB, (BRANCH_WIDTH,) * 4)
    q = jax.nn.silu(q)
    log_f = jnp.logaddexp(jnp.log(lb), jnp.log1p(-lb) + jax.nn.log_sigmoid(f))
    k = (1.0 - lb) * jax.nn.sigmoid(-f)
    o = _gla_chunked(*(_to_heads(t, HGRN_HEADS) for t in (q, k, i, log_f)))
    o = _head_rmsnorm(_from_heads(o), NORM_EPS) * norm_w
    return o * jax.nn.silu(g)


def _mlstm_chunked(q, k, v, i_pre, log_f):
    bsz, nh, _, dk = q.shape
    dv = v.shape[-1]
    causal = jnp.tril(jnp.ones((CHUNK, CHUNK), bool))

    def chunk(carry, inp):
        c_mat, n_vec, m_prev = carry
        qc, kc, vc, ic, lf = inp
        fcum = jnp.cumsum(lf, axis=-1)
        log_w = jnp.where(causal, fcum[..., :, None] - fcum[..., None, :] + ic[..., None, :], -jnp.inf)
        log_inter = fcum + m_prev[..., None]
        m_t = jnp.maximum(log_inter, jnp.max(log_w, -1))
        w_intra = jnp.exp(log_w - m_t[..., None])
        w_inter = jnp.exp(log_inter - m_t)
        s_qk = jnp.einsum('bhtd,bhsd->bhts', qc, kc) * w_intra
        num = w_inter[..., None] * jnp.einsum('bhtd,bhde->bhte', qc, c_mat) + jnp.einsum('bhts,bhse->bhte', s_qk, vc)
        den = w_inter * jnp.einsum('bhtd,bhd->bht', qc, n_vec) + jnp.sum(s_qk, -1)
        h = num / jnp.maximum(jnp.abs(den), jnp.exp(-m_t))[..., None]
        f_last = fcum[..., -1]
        log_s = f_last[..., None] - fcum + ic
        m_new = jnp.maximum(f_last + m_prev, jnp.max(log_s, -1))
        ws = jnp.exp(log_s - m_new[..., None])
        carry_decay = jnp.exp(f_last + m_prev - m_new)
        c_mat = carry_decay[..., None, None] * c_mat + jnp.einsum('bhs,bhsd,bhse->bhde', ws, kc, vc)
        n_vec = carry_decay[..., None] * n_vec + jnp.einsum('bhs,bhsd->bhd', ws, kc)
        return (c_mat, n_vec, m_new), h

    carry0 = (jnp.zeros((bsz, nh, dk, dv), F32), jnp.zeros((bsz, nh, dk), F32), jnp.full((bsz, nh), -1e30, F32))
    _, h = lax.scan(chunk, carry0, tuple(_to_chunks(t) for t in (q, k, v, i_pre, log_f)))
    return _from_chunks(h)


def mlstm_mix(zC, conv_w, conv_b, i_bias, f_bias, norm_w):
    q, k, v, o, ig, fg = _split(zC, MLSTM_SPLITS)
    qk = jax.nn.silu(_causal_conv(jnp.concatenate([q, k], -1), conv_w, conv_b))
    q, k = _split(qk, (BRANCH_WIDTH, BRANCH_WIDTH))
    qh = _to_heads(q, MLSTM_HEADS)
    kh = _to_heads(k, MLSTM_HEADS) * MLSTM_HEAD_DIM ** -0.5
    vh = _to_heads(v, MLSTM_HEADS)
    i_pre = (ig + i_bias).transpose(0, 2, 1)
    log_f = jax.nn.log_sigmoid(fg + f_bias).transpose(0, 2, 1)
    h = _mlstm_chunked(qh, kh, vh, i_pre, log_f)
    h = _head_layernorm(_from_heads(h), NORM_EPS) * norm_w
    return jax.nn.sigmoid(o) * h


def _linear_combine(left, right):
    a_l, b_l = left
    a_r, b_r = right
    return a_l * a_r, a_r * b_l + b_r


def rglru_mix(zD, conv_w, conv_b, wx, bx, wa, ba, lam):
    xr, gate = _split(zD, (BRANCH_WIDTH, BRANCH_WIDTH))
    xc = _causal_conv(xr, conv_w, conv_b)
    bsz, seq = xc.shape[:2]
    xb = xc.reshape(bsz, seq, LRU_BLOCKS, LRU_BLOCK_DIM)
    gate_x = jax.nn.sigmoid(jnp.einsum('bsgi,gij->bsgj', xb, wx) + bx).reshape(bsz, seq, BRANCH_WIDTH)
    gate_a = jax.nn.sigmoid(jnp.einsum('bsgi,gij->bsgj', xb, wa) + ba).reshape(bsz, seq, BRANCH_WIDTH)
    log_a = LRU_C * gate_a * jax.nn.log_sigmoid(lam)
    mult = jnp.sqrt(-jnp.expm1(2.0 * log_a)).at[:, 0].set(1.0)
    _, h = lax.associative_scan(_linear_combine, (jnp.exp(log_a), mult * gate_x * xc), axis=1)
    return h * jax.nn.gelu(gate)


def _expert_ffn(xb, w_gu, b_gu, w_down, b_down):
    gu = xb @ w_gu + b_gu
    gate, up = gu[:, :D_EXPERT], gu[:, D_EXPERT:]
    gate = jnp.minimum(gate, SWIGLU_LIMIT)
    up = jnp.clip(up, -SWIGLU_LIMIT, SWIGLU_LIMIT)
    glu = gate * jax.nn.sigmoid(SWIGLU_ALPHA * gate)
    return ((up + 1.0) * glu) @ w_down + b_down


def moe(xf, router_w, router_b, w_gu, b_gu, w_down, b_down):
    n_tok = xf.shape[0]
    logits = (xf @ router_w + router_b).astype(F32)
    top_val, top_idx = lax.top_k(logits, TOP_K)
    gate = jax.nn.softmax(top_val, axis=-1)
    n_assign = n_tok * TOP_K
    n_blocks = -(-n_assign // EXPERT_BLOCK) + N_EXPERTS
    n_slots = n_blocks * EXPERT_BLOCK
    e_flat = top_idx.reshape(-1)
    order = jnp.argsort(e_flat)
    e_sorted = e_flat[order]
    counts = jnp.bincount(e_flat, length=N_EXPERTS)
    padded = (counts + EXPERT_BLOCK - 1) // EXPERT_BLOCK * EXPERT_BLOCK
    start = jnp.cumsum(counts) - counts
    pad_end = jnp.cumsum(padded)
    pad_start = pad_end - padded
    dest = pad_start[e_sorted] + jnp.arange(n_assign) - start[e_sorted]
    slot_tok = jnp.zeros((n_slots,), jnp.int32).at[dest].set((order // TOP_K).astype(jnp.int32))
    slot_w = jnp.zeros((n_slots,), F32).at[dest].set(gate.reshape(-1)[order])
    block_e = jnp.minimum(jnp.searchsorted(pad_end, jnp.arange(n_blocks) * EXPERT_BLOCK, side='right'), N_EXPERTS - 1)
    xs = xf[slot_tok].reshape(n_blocks, EXPERT_BLOCK, -1)

    def run_block(args):
        xb, e = args
        return _expert_ffn(xb, w_gu[e], b_gu[e], w_down[e], b_down[e])

    ys = lax.map(run_block, (xs, block_e)).reshape(n_slots, -1)
    return jnp.zeros(xf.shape, F32).at[slot_tok].add(ys.astype(F32) * slot_w[:, None])


def setup_inputs(seed: int = 0) -> dict:
    key = jax.random.key(seed)
    ks = iter(jax.random.split(key, 64))
    L, W, D = DEPTH, BRANCH_WIDTH, D_MODEL

    def nrm(shape, scale):
        return scale * jax.random.normal(next(ks), shape, F32)

    def uni(shape, lo, hi):
        return jax.random.uniform(next(ks), shape, F32, lo, hi)

    return {
        'x': nrm((BATCH, SEQ, D), 1.0),
        'p': nrm((L, BATCH, SEQ, D_PLE), 1.0),
        'w_in_first': nrm((D, N_COLS_BASE), D ** -0.5),
        'w_in_rest': nrm((L - 1, D, N_COLS_REST), D ** -0.5),
        'rwkv_mu': uni((L, RWKV_COLS), 0.0, 1.0),
        'rwkv_w0': uni((L, W), -6.0, 0.0),
        'rwkv_w2': nrm((L, RWKV_DECAY_RANK, W), 0.5 * RWKV_DECAY_RANK ** -0.5),
        'rwkv_a0': nrm((L, W), 0.1),
        'rwkv_a2': nrm((L, RWKV_ICL_RANK, W), 0.5 * RWKV_ICL_RANK ** -0.5),
        'rwkv_v0': nrm((L - 1, W), 0.1),
        'rwkv_v2': nrm((L - 1, RWKV_VALUE_RANK, W), 0.5 * RWKV_VALUE_RANK ** -0.5),
        'rwkv_g2': nrm((L, RWKV_GATE_RANK, W), RWKV_GATE_RANK ** -0.5),
        'rwkv_k_k': 0.85 + nrm((L, W), 0.05),
        'rwkv_k_a': 1.0 + nrm((L, W), 0.05),
        'rwkv_r_k': nrm((L, RWKV_HEADS, RWKV_HEAD_DIM), 0.1),
        'rwkv_lnx_g': 1.0 + nrm((L, W), 0.05),
        'rwkv_lnx_b': nrm((L, W), 0.02),
        'hgrn_lower_bounds': nrm((L, W), 1.0),
        'hgrn_norm_w': 1.0 + nrm((L, W), 0.05),
        'mlstm_conv_w': nrm((L, CONV_WIDTH, 2 * W), CONV_WIDTH ** -0.5),
        'mlstm_conv_b': nrm((L, 2 * W), 0.02),
        'mlstm_i_bias': nrm((L, MLSTM_HEADS), 0.1),
        'mlstm_f_bias': jnp.linspace(3.0, 6.0, MLSTM_HEADS, dtype=F32) + nrm((L, MLSTM_HEADS), 0.1),
        'mlstm_norm_w': 1.0 + nrm((L, W), 0.05),
        'lru_conv_w': nrm((L, CONV_WIDTH, W), CONV_WIDTH ** -0.5),
        'lru_conv_b': nrm((L, W), 0.02),
        'lru_wx': nrm((L, LRU_BLOCKS, LRU_BLOCK_DIM, LRU_BLOCK_DIM), LRU_BLOCK_DIM ** -0.5),
        'lru_bx': nrm((L, LRU_BLOCKS, LRU_BLOCK_DIM), 0.02),
        'lru_wa': nrm((L, LRU_BLOCKS, LRU_BLOCK_DIM, LRU_BLOCK_DIM), LRU_BLOCK_DIM ** -0.5),
        'lru_ba': nrm((L, LRU_BLOCKS, LRU_BLOCK_DIM), 0.02),
        'lru_lambda': uni((L, W), 3.5, 9.0),
        'w_branch': nrm((L, N_BRANCH, W, D), W ** -0.5),
        'w_out': nrm((L, D, D), DEEPNORM_BETA * D ** -0.5),
        'ln1_g': 1.0 + nrm((L, D), 0.05),
        'ln1_b': nrm((L, D), 0.02),
        'router_w': nrm((L, D, N_EXPERTS), D ** -0.5),
        'router_b': nrm((L, N_EXPERTS), 0.01),
        'expert_w_gu': nrm((L, N_EXPERTS, D, 2 * D_EXPERT), D ** -0.5),
        'expert_b_gu': nrm((L, N_EXPERTS, 2 * D_EXPERT), 0.02),
        'expert_w_down': nrm((L, N_EXPERTS, D_EXPERT, D), DEEPNORM_BETA * D_EXPERT ** -0.5),
        'expert_b_down': nrm((L, N_EXPERTS, D), 0.02),
        'ple_gate_w': nrm((L, D, D), D ** -0.5),
        'ple_proj_w': nrm((L, D_PLE, D), DEEPNORM_BETA * D_PLE ** -0.5),
        'ln2_g': 1.0 + nrm((L, D), 0.05),
        'ln2_b': nrm((L, D), 0.02),
    }


def reference(x, p, w_in_first, w_in_rest, rwkv_mu, rwkv_w0, rwkv_w2, rwkv_a0, rwkv_a2, rwkv_v0, rwkv_v2,
              rwkv_g2, rwkv_k_k, rwkv_k_a, rwkv_r_k, rwkv_lnx_g, rwkv_lnx_b, hgrn_lower_bounds, hgrn_norm_w,
              mlstm_conv_w, mlstm_conv_b, mlstm_i_bias, mlstm_f_bias, mlstm_norm_w, lru_conv_w, lru_conv_b,
              lru_wx, lru_bx, lru_wa, lru_ba, lru_lambda, w_branch, w_out, ln1_g, ln1_b, router_w, router_b,
              expert_w_gu, expert_b_gu, expert_w_down, expert_b_down, ple_gate_w, ple_proj_w, ln2_g, ln2_b):
    dt = x.dtype
    bsz, seq, _ = x.shape
    lb_all = jnp.cumsum(jax.nn.softmax(hgrn_lower_bounds.astype(F32), axis=0), axis=0)
    lb_all = lb_all - lb_all[0]
    v_first = None
    for layer in range(DEPTH):
        w_in = w_in_first if layer == 0 else w_in_rest[layer - 1]
        z = (x @ w_in).astype(F32)
        zA, zB, zC, zD, zG = _split(z[..., :N_COLS_BASE], (RWKV_COLS, HGRN_COLS, MLSTM_COLS, LRU_COLS, GATE_COLS))
        v_mix = None if layer == 0 else rwkv_v0[layer - 1] + z[..., N_COLS_BASE:] @ rwkv_v2[layer - 1]
        yA, v_cur = rwkv7_mix(zA, v_first, v_mix, rwkv_mu[layer], rwkv_w0[layer], rwkv_w2[layer], rwkv_a0[layer],
                              rwkv_a2[layer], rwkv_g2[layer], rwkv_k_k[layer], rwkv_k_a[layer], rwkv_r_k[layer],
                              rwkv_lnx_g[layer], rwkv_lnx_b[layer])
        if layer == 0:
            v_first = v_cur
        yB = hgrn2_mix(zB, lb_all[layer], hgrn_norm_w[layer])
        yC = mlstm_mix(zC, mlstm_conv_w[layer], mlstm_conv_b[layer], mlstm_i_bias[layer], mlstm_f_bias[layer],
                       mlstm_norm_w[layer])
        yD = rglru_mix(zD, lru_conv_w[layer], lru_conv_b[layer], lru_wx[layer], lru_bx[layer], lru_wa[layer],
                       lru_ba[layer], lru_lambda[layer])
        gates = jax.nn.sigmoid(zG).reshape(bsz, seq, N_BRANCH, D_MODEL)
        merged = jnp.zeros((bsz, seq, D_MODEL), F32)
        for n, yb in enumerate((yA, yB, yC, yD)):
            merged = merged + gates[:, :, n] * (yb @ w_branch[layer, n])
        mix = merged @ w_out[layer]
        x = _layernorm(DEEPNORM_ALPHA * x + mix, ln1_g[layer], ln1_b[layer], dt)
        moe_out = moe(x.reshape(bsz * seq, D_MODEL), router_w[layer], router_b[layer], expert_w_gu[layer],
                      expert_b_gu[layer], expert_w_down[layer], expert_b_down[layer]).reshape(bsz, seq, D_MODEL)
        ple = jax.nn.sigmoid(x @ ple_gate_w[layer]) * (p[layer] @ ple_proj_w[layer])
        x = _layernorm(DEEPNORM_ALPHA * x + moe_out + ple, ln2_g[layer], ln2_b[layer], dt)
    return x
```

```python
import contextlib
import numpy as np
import concourse.bass as bass
import concourse.mybir as mybir
from concourse.bass_utils import run_bass_kernel_spmd

F32 = mybir.dt.float32
BF16 = mybir.dt.bfloat16
AF = mybir.ActivationFunctionType
ALU = mybir.AluOpType
AX = mybir.AxisListType

D = 1024
SEQ = 2048
DEPTH = 4
NT = SEQ // 128
W = 512
NEXP = 32
DPLE = 256
ALPHA = float((2 * DEPTH) ** 0.25)
LN_EPS = 1e-5
NORM_EPS = 1e-6
RWKV_COLS = 1792
N_COLS_BASE = 11016
N_COLS_REST = 11048
TB = 512
NB = SEQ // TB
TBA = 256
NBA = SEQ // TBA

ENG_NAMES = ["pe", "act", "dve", "pool", "sp"]


class Buf:
    __slots__ = ("name", "w", "r", "excl")

    def __init__(self, name="", excl=False):
        self.name = name
        self.w = {}
        self.r = {}
        self.excl = excl


class FW:
    def __init__(self, nc, sems, n_dma_sems):
        self.nc = nc
        self.sems = sems
        self.ops = {e: [] for e in ENG_NAMES}
        self.cnt = {e: 0 for e in ENG_NAMES}
        self.waited = {e: {} for e in ENG_NAMES}
        self.n_dma_sems = n_dma_sems
        self.dma_cnt = [0] * n_dma_sems
        half = n_dma_sems // 2
        self.dma_rng = {"sp": (0, half), "act": (0, half), "pool": (half, n_dma_sems)}
        self.dma_rr = {"sp": 0, "act": 0, "pool": half}
        self.n_instr = 0
        self.final_tokens = []

    def _need(self, eng, deps):
        for key, val in deps.items():
            if key == ("e", "pe") and eng == "pe":
                continue
            if self.waited[eng].get(key, 0) >= val:
                continue
            self.waited[eng][key] = val
            self.ops[eng].append(("wait", key, val))

    @staticmethod
    def _collect(reads, writes):
        deps = {}
        for b in reads:
            for k, v in b.w.items():
                if deps.get(k, 0) < v:
                    deps[k] = v
        for b in writes:
            for k, v in b.w.items():
                if deps.get(k, 0) < v:
                    deps[k] = v
            for k, v in b.r.items():
                if deps.get(k, 0) < v:
                    deps[k] = v
        return deps

    @staticmethod
    def _mark(tok, reads, writes):
        k, v = tok
        for b in reads:
            if b.r.get(k, 0) < v:
                b.r[k] = v
        for b in writes:
            if b.w.get(k, 0) < v:
                b.w[k] = v

    def op(self, eng, fn, reads=(), writes=()):
        ex = [b for b in reads if b.excl]
        if ex:
            reads = [b for b in reads if not b.excl]
            writes = list(writes) + ex
        self._need(eng, self._collect(reads, writes))
        self.cnt[eng] += 1
        tok = (("e", eng), self.cnt[eng])
        self.ops[eng].append(("op", fn))
        self._mark(tok, reads, writes)
        self.n_instr += 1
        return tok

    def dma(self, q, out, in_, reads=(), writes=(), final=False):
        deps = self._collect(reads, writes)
        lo, hi = self.dma_rng[q]
        rk = "pool" if q == "pool" else "sp"
        idx = self.dma_rr[rk]
        self.dma_rr[rk] = lo + (idx + 1 - lo) % (hi - lo)
        key = ("d", idx)
        if self.dma_cnt[idx] > 0:
            prev = self.dma_cnt[idx] * 16
            if deps.get(key, 0) < prev:
                deps[key] = prev
        self._need(q, deps)
        self.dma_cnt[idx] += 1
        tok = (key, self.dma_cnt[idx] * 16)
        self.ops[q].append(("dma", out, in_, key))
        self._mark(tok, reads, writes)
        if final:
            self.final_tokens.append(tok)
        self.n_instr += 1
        return tok

    def flush(self, final=False):
        allv = {("e", e): self.cnt[e] for e in ENG_NAMES if self.cnt[e] > 0}
        for i in range(self.n_dma_sems):
            if self.dma_cnt[i] > 0:
                allv[("d", i)] = self.dma_cnt[i] * 16
        for e in ENG_NAMES:
            d = dict(allv)
            d.pop(("e", e), None)
            self._need(e, d)
        nc = self.nc
        sems = self.sems
        with nc.Block() as block:
            regs = {"pe": block.tensor, "act": block.scalar, "dve": block.vector,
                    "pool": block.gpsimd, "sp": block.sync}

            def make(e):
                items = self.ops[e]

                def body(eng):
                    mysem = sems[("e", e)]
                    for item in items:
                        kind = item[0]
                        if kind == "wait":
                            eng.wait_ge(sems[item[1]], item[2])
                        elif kind == "op":
                            item[1](eng).then_inc(mysem, 1)
                        else:
                            eng.dma_start(out=item[1], in_=item[2]).then_inc(sems[item[3]], 16)
                return body
            for e in ENG_NAMES:
                if self.ops[e]:
                    regs[e](make(e))
        self.ops = {e: [] for e in ENG_NAMES}


class T:
    def __init__(self, t, excl=False):
        self.t = t
        self.bufs = {}
        self.excl = excl

    def b(self, key=0):
        if key not in self.bufs:
            self.bufs[key] = Buf(excl=self.excl)
        return self.bufs[key]

    def __getitem__(self, idx):
        return self.t[idx]


class K:
    def __init__(self, cfg):
        self.cfg = cfg
        self.nc = bass.Bass("TRN2", target_bir_lowering=False)
        self.st = contextlib.ExitStack()
        self.dram = {}
        self.NDS = 32

    def din(self, name, shape, dtype=F32):
        self.dram[name] = self.nc.dram_tensor(name, list(shape), dtype, kind="ExternalInput").ap()
        return self.dram[name]

    def dout(self, name, shape, dtype=F32):
        self.dram[name] = self.nc.dram_tensor(name, list(shape), dtype, kind="ExternalOutput").ap()
        return self.dram[name]

    def sb(self, name, shape, dtype=F32, stack=None):
        t = (stack or self.st).enter_context(self.nc.sbuf_tensor(name, list(shape), dtype))
        return T(t)

    def setup(self):
        nc = self.nc
        st = self.st
        sems = {}
        for e in ENG_NAMES:
            sems[("e", e)] = st.enter_context(nc.semaphore("s_" + e))
        for i in range(self.NDS):
            sems[("d", i)] = st.enter_context(nc.semaphore("sd%d" % i))
        self.fw = FW(nc, sems, self.NDS)
        self.psum = []
        for i in range(8):
            p = st.enter_context(nc.psum_tensor("ps%d" % i, [128, 512], F32))
            self.psum.append(T(p, excl=True))
        self.ps_rr = 0

    def ps(self):
        p = self.psum[self.ps_rr]
        self.ps_rr = (self.ps_rr + 1) % 8
        return p

    def mm(self, out, lhsT, rhs, start, stop, reads, writes):
        self.fw.op("pe", lambda e: e.matmul(out, lhsT=lhsT, rhs=rhs, start=start, stop=stop), reads, writes)

    def tr(self, out, in_, ident, reads, writes):
        self.fw.op("pe", lambda e: e.transpose(out, in_, ident), reads, writes)

    def act(self, out, in_, func, reads, writes, bias=None, scale=None, eng="act", accum_out=None):
        kw = {}
        if bias is not None:
            kw["bias"] = bias
        if scale is not None:
            kw["scale"] = scale
        if accum_out is not None:
            kw["accum_out"] = accum_out
        self.fw.op("act", lambda e: e.activation(out=out, in_=in_, func=func, **kw), reads, writes)

    def ts(self, eng, out, in0, s1, s2, op0, op1, reads, writes):
        if op1 is None:
            self.fw.op(eng, lambda e: e.tensor_scalar(out=out, in0=in0, scalar1=s1, scalar2=None, op0=op0), reads, writes)
        else:
            self.fw.op(eng, lambda e: e.tensor_scalar(out=out, in0=in0, scalar1=s1, scalar2=s2, op0=op0, op1=op1), reads, writes)

    def tt(self, eng, out, in0, in1, op, reads, writes):
        self.fw.op(eng, lambda e: e.tensor_tensor(out=out, in0=in0, in1=in1, op=op), reads, writes)

    def stt(self, out, in0, scalar, in1, op0, op1, reads, writes):
        self.fw.op("dve", lambda e: e.scalar_tensor_tensor(out=out, in0=in0, scalar=scalar, in1=in1, op0=op0, op1=op1), reads, writes)

    def cp(self, eng, out, in_, reads, writes):
        if eng == "act":
            self.fw.op("act", lambda e: e.activation(out=out, in_=in_, func=AF.Copy), reads, writes)
        else:
            self.fw.op(eng, lambda e: e.tensor_copy(out=out, in_=in_), reads, writes)


def _cols(v):
    v = np.asarray(v, np.float32)
    return np.ascontiguousarray(v.reshape(-1, 128).T)


def host_consts():
    c = {}
    c["ident"] = np.eye(128, dtype=np.float32)
    s = np.arange(64)
    c["m_incl"] = np.tile((s[:, None] <= s[None, :]).astype(np.float32), (1, 8))
    c["m_strict"] = np.tile((s[:, None] < s[None, :]).astype(np.float32), (1, 8))
    c["m_lows"] = np.tile((s[:, None] > s[None, :]).astype(np.float32), (1, 8))
    c["ones"] = np.ones((128, 128), np.float32)
    bd = np.zeros((128, 128), np.float32)
    bd[:64, :64] = 1.0
    bd[64:, 64:] = 1.0
    c["bd64"] = bd
    rm = np.ones((128, 512), np.float32)
    rm[:, ::64] = 0.0
    c["rmask"] = rm
    return c


def build_inmaps(inputs):
    f = lambda a: np.ascontiguousarray(np.asarray(a, np.float32))
    L = DEPTH
    shared = {}
    shared["w_in_first"] = f(inputs["w_in_first"])
    shared["w_in_rest"] = f(inputs["w_in_rest"]).reshape(3 * D, N_COLS_REST)
    shared["w_gu"] = f(inputs["expert_w_gu"]).reshape(-1, 2 * D)
    shared["w_dn"] = f(inputs["expert_w_down"]).reshape(-1, D)
    shared["b_dn"] = f(inputs["expert_b_down"]).reshape(L * NEXP, D)
    bgu = f(inputs["expert_b_gu"]).reshape(L, NEXP, 16, 128)
    shared["b_gu"] = np.ascontiguousarray(bgu.transpose(0, 3, 1, 2)).reshape(L * 128, NEXP * 16)
    shared["w_pg"] = f(inputs["ple_gate_w"]).reshape(L * D, D)
    shared["w_pp"] = f(inputs["ple_proj_w"]).reshape(L * DPLE, D)
    shared["w_out"] = f(inputs["w_out"]).reshape(L * D, D)
    shared["w_br"] = f(inputs["w_branch"]).reshape(L * 4 * W, D)
    shared["w_rt"] = f(inputs["router_w"]).reshape(L * D, NEXP)
    shared["b_rt"] = f(inputs["router_b"])
    shared["ln1_g"] = f(inputs["ln1_g"]); shared["ln1_b"] = f(inputs["ln1_b"])
    shared["ln2_g"] = f(inputs["ln2_g"]); shared["ln2_b"] = f(inputs["ln2_b"])
    for k, v in host_consts().items():
        shared["c_" + k] = v
    build_inmaps_a(inputs, shared)
    maps = []
    for b in range(8):
        m = dict(shared)
        m["x"] = f(inputs["x"][b])
        m["pT"] = np.ascontiguousarray(f(inputs["p"][:, b]).transpose(0, 2, 1)).reshape(L * DPLE, SEQ)
        maps.append(m)
    return maps


def declare_dram(k):
    L = DEPTH
    LE = k.cfg.get("declLE", L * NEXP)
    k.din("x", [SEQ, D]); k.din("pT", [L * DPLE, SEQ])
    k.din("w_in_first", [D, N_COLS_BASE]); k.din("w_in_rest", [k.cfg.get("declR", 3) * D, N_COLS_REST])
    k.din("w_gu", [LE * D, 2 * D]); k.din("w_dn", [LE * D, D]); k.din("b_dn", [L * NEXP, D])
    k.din("b_gu", [L * 128, NEXP * 16])
    k.din("w_pg", [L * D, D]); k.din("w_pp", [L * DPLE, D]); k.din("w_out", [L * D, D]); k.din("w_br", [L * 4 * W, D])
    k.din("w_rt", [L * D, NEXP]); k.din("b_rt", [L, NEXP])
    for n in ("ln1_g", "ln1_b", "ln2_g", "ln2_b"):
        k.din(n, [L, D])
    for n, v in host_consts().items():
        k.din("c_" + n, list(v.shape))
    declare_dram_a(k)
    k.dout("y", [SEQ, D])


def bcast_rows(ap_row, nparts):
    return ap_row.partition_broadcast(nparts) if hasattr(ap_row, "partition_broadcast") else ap_row


def load_consts(k):
    fw = k.fw
    C = {}
    for n, v in host_consts().items():
        t = k.sb("sc_" + n, list(v.shape))
        fw.dma("sp", t[:], k.dram["c_" + n], writes=[t.b()])
        C[n] = t
    k.C = C


def make_xT(k, tile, layer, with_router):
    fw = k.fw
    X, XT, C = k.X, k.XT, k.C
    tok = slice(tile * 128, (tile + 1) * 128)
    xtf = k.B_xtf if with_router else None
    for hb in range(2):
        p = k.ps()
        for cc in range(4):
            c = hb * 4 + cc
            k.tr(p[:, cc * 128:(cc + 1) * 128], X[:, tile, c * 128:(c + 1) * 128], C["ident"][:],
                 [X.b(tile), C["ident"].b()], [p.b()])
        src = p[:, :].rearrange("p (c t) -> p c t", c=4)
        k.cp("act", XT[:, hb * 4:(hb + 1) * 4, tok], src, [p.b()], [XT.b(tile)])
        if with_router:
            k.cp("dve", xtf[:, hb * 4:(hb + 1) * 4, :], src, [p.b()], [xtf.b()])
    xtv = k.cfg.get("xtv", "")
    if with_router and "nomm" not in xtv:
        p = k.ps()
        for c in range(8):
            k.mm(p[:, 0:NEXP], xtf[:, c, :], k.B_wrt[:, c, :], c == 0, c == 7, [xtf.b(), k.B_wrt.b()], [p.b()])
        if "nott" not in xtv:
            k.tt("dve", k.B_lg[:, tile, :], p[:, 0:NEXP], k.B_brt[:], ALU.add, [p.b(), k.B_brt.b()], [k.B_lg.b(tile)])


def wslot(k):
    s = k.ring_rr
    k.ring_rr = (k.ring_rr + 1) % k.NRING
    return s


def load_w(k, dram_ap_2d, kc, ncols, q="pool"):
    s = wslot(k)
    full = k.ring[s]
    view = full[:, 0:kc * ncols].rearrange("p (a c) -> p a c", a=kc)
    src = dram_ap_2d.rearrange("(a p) c -> p a c", p=128)
    k.fw.dma(q, view, src, writes=[k.ringb[s]])
    return view, k.ringb[s]


def phase_b(k, layer, last):
    fw = k.fw
    nc = k.nc
    X, XT, C = k.X, k.XT, k.C
    st = contextlib.ExitStack()
    dr = k.dram
    with st:
        k.NRING = 4
        k.ring = []
        k.ringb = []
        for i in range(k.NRING):
            t = st.enter_context(nc.sbuf_tensor("ringB%d_%d" % (layer, i), [128, 4096], BF16))
            k.ring.append(t)
            k.ringb.append(Buf())
        k.ring_rr = 0
        HT = k.sb("HT%d" % layer, [128, 8, SEQ], BF16, st)
        G = k.sb("G%d" % layer, [128, NT, NEXP], F32, st)
        G2 = k.sb("G2_%d" % layer, [128, D], F32, st)
        B2 = k.sb("B2_%d" % layer, [128, D], F32, st)
        BD = k.sb("BD%d" % layer, [NEXP, D], F32, st)
        BGU = k.sb("BGU%d" % layer, [128, NEXP * 16], F32, st)
        BU1 = k.sb("BU1_%d" % layer, [128, NEXP * 8], F32, st)
        NTMP = 6
        tmp = [k.sb("tmpB%d_%d" % (layer, i), [128, 512], F32, st) for i in range(NTMP)]
        sm = k.sb("smB%d" % layer, [128, 64], F32, st)
        tstate = {"i": 0}

        def T_():
            t = tmp[tstate["i"]]
            tstate["i"] = (tstate["i"] + 1) % NTMP
            return t

        fw.dma("sp", G2[:], dr["ln2_g"][layer:layer + 1, :].partition_broadcast(128), writes=[G2.b()])
        fw.dma("sp", B2[:], dr["ln2_b"][layer:layer + 1, :].partition_broadcast(128), writes=[B2.b()])
        fw.dma("sp", BD[:], dr["b_dn"][layer * NEXP:(layer + 1) * NEXP, :], writes=[BD.b()])
        fw.dma("sp", BGU[:], dr["b_gu"][layer * 128:(layer + 1) * 128, :], writes=[BGU.b()])
        bgu3 = BGU[:, :].rearrange("p (e c) -> p e c", c=16)
        k.ts("dve", BU1[:, :].rearrange("p (e c) -> p e c", c=8), bgu3[:, :, 8:16], 1.0, None, ALU.add, None,
             [BGU.b()], [BU1.b()])

        LG = k.B_lg
        skip = k.cfg.get('skip', ())
        for tile in range(NT if 'router' not in skip else 0):
            m8 = sm[:, 0:8]
            fw.op("dve", lambda e, tile=tile: e.max(out=sm[:, 0:8], in_=LG[:, tile, :]), [LG.b(tile)], [sm.b()])
            mask = T_()
            k.ts("dve", mask[:, 0:NEXP], LG[:, tile, :], sm[:, 3:4], None, ALU.is_ge, None, [LG.b(tile), sm.b()], [mask.b()])
            k.ts("dve", sm[:, 8:9], sm[:, 0:1], -1.0, None, ALU.mult, None, [sm.b()], [sm.b()])
            ex = T_()
            k.act(ex[:, 0:NEXP], LG[:, tile, :], AF.Exp, [LG.b(tile), sm.b()], [ex.b()], bias=sm[:, 8:9])
            k.tt("dve", ex[:, 0:NEXP], ex[:, 0:NEXP], mask[:, 0:NEXP], ALU.mult, [ex.b(), mask.b()], [ex.b()])
            fw.op("dve", lambda e, ex=ex: e.reduce_sum(out=sm[:, 9:10], in_=ex[:, 0:NEXP], axis=AX.X), [ex.b()], [sm.b()])
            fw.op("dve", lambda e: e.reciprocal(out=sm[:, 10:11], in_=sm[:, 9:10]), [sm.b()], [sm.b()])
            k.ts("dve", G[:, tile, :], ex[:, 0:NEXP], sm[:, 10:11], None, ALU.mult, None, [ex.b(), sm.b()], [G.b(tile)])

        for half in range(2 if 'ple' not in skip else 0):
            cs = slice(half * 512, (half + 1) * 512)
            wpg, bpg = load_w(k, dr["w_pg"][layer * D:(layer + 1) * D, cs], 8, 512)
            wpp, bpp = load_w(k, dr["w_pp"][layer * DPLE:(layer + 1) * DPLE, cs], 2, 512)
            ptv, bpt = load_w(k, dr["pT"][layer * DPLE:(layer + 1) * DPLE, :], 2, SEQ)
            for tile in range(NT):
                tok = slice(tile * 128, (tile + 1) * 128)
                pg = k.ps()
                for c in range(8):
                    k.mm(pg[:, :], XT[:, c, tok], wpg[:, c, :], c == 0, c == 7, [XT.b(tile), bpg], [pg.b()])
                pp = k.ps()
                for c in range(2):
                    k.mm(pp[:, :], ptv[:, c, tok], wpp[:, c, :], c == 0, c == 1, [bpt, bpp], [pp.b()])
                sg = T_()
                k.act(sg[:], pg[:, :], AF.Sigmoid, [pg.b()], [sg.b()])
                k.tt("dve", sg[:], sg[:], pp[:, :], ALU.mult, [sg.b(), pp.b()], [sg.b()])
                k.stt(X[:, tile, cs], X[:, tile, cs], ALPHA, sg[:], ALU.mult, ALU.add, [X.b(tile), sg.b()], [X.b(tile)])

        nexp = k.cfg.get("nexp", NEXP)
        for e in range(nexp):
            r0 = (layer * NEXP + e) * D
            wgu = dr["w_gu"][r0:r0 + D, :]
            wdn = dr["w_dn"][r0:r0 + D, :]
            for hp in range(2):
                wg, bg = load_w(k, wgu[:, hp * 512:(hp + 1) * 512], 8, 512)
                wu, bu = load_w(k, wgu[:, D + hp * 512:D + (hp + 1) * 512], 8, 512)
                for tb in range(NB):
                    ts_ = slice(tb * TB, (tb + 1) * TB)
                    xb = [XT.b(tb * 4 + i) for i in range(4)]
                    for jj in range(4):
                        j = hp * 4 + jj
                        js = slice(jj * 128, (jj + 1) * 128)
                        pG = k.ps()
                        for c in range(8):
                            k.mm(pG[:, :], wg[:, c, js], XT[:, c, ts_], c == 0, c == 7, xb + [bg], [pG.b()])
                        pU = k.ps()
                        for c in range(8):
                            k.mm(pU[:, :], wu[:, c, js], XT[:, c, ts_], c == 0, c == 7, xb + [bu], [pU.b()])
                        g = T_()
                        k.ts("dve", g[:], pG[:, :], BGU[:, e * 16 + j:e * 16 + j + 1], 7.0, ALU.add, ALU.min, [pG.b(), BGU.b()], [g.b()])
                        s = T_()
                        k.act(s[:], g[:], AF.Sigmoid, [g.b()], [s.b()], scale=1.702)
                        u1 = T_()
                        k.ts("dve", u1[:], pU[:, :], BU1[:, e * 8 + j:e * 8 + j + 1], 8.0, ALU.add, ALU.min, [pU.b(), BU1.b()], [u1.b()])
                        k.tt("pool", g[:], g[:], s[:], ALU.mult, [g.b(), s.b()], [g.b()])
                        k.stt(HT[:, j, ts_], u1[:], -6.0, g[:], ALU.max, ALU.mult, [u1.b(), g.b()], [HT.b(tb)])
            wd = []
            for half in range(2):
                wd.append(load_w(k, wdn[:, half * 512:(half + 1) * 512], 8, 512))
            for tile in range(NT):
                tok = slice(tile * 128, (tile + 1) * 128)
                for half in range(2):
                    cs = slice(half * 512, (half + 1) * 512)
                    pY = k.ps()
                    for c in range(8):
                        k.mm(pY[:, :], HT[:, c, tok], wd[half][0][:, c, :], c == 0, c == 7, [HT.b(tile // 4), wd[half][1]], [pY.b()])
                    k.stt(X[:, tile, cs], pY[:, :], G[:, tile, e:e + 1], X[:, tile, cs], ALU.mult, ALU.add,
                          [pY.b(), G.b(tile), X.b(tile)], [X.b(tile)])

        gt = k.sb("gtB%d" % layer, [NEXP, 128], F32, st)
        stt_ = k.sb("stB%d" % layer, [128, 16], F32, st)
        for tile in range(NT):
            if 'tail' in skip:
                fw.dma('sp', dr['y'][tile * 128:(tile + 1) * 128, :], X[:, tile, :], reads=[X.b(tile)], final=True)
                continue
            p = k.ps()
            k.tr(p[0:NEXP, 0:128], G[:, tile, :], C["ident"][:], [G.b(tile), C["ident"].b()], [p.b()])
            k.cp("act", gt[:], p[0:NEXP, 0:128], [p.b()], [gt.b()])
            for half in range(2):
                cs = slice(half * 512, (half + 1) * 512)
                pb = k.ps()
                k.mm(pb[:, :], gt[:], BD[:, cs], True, True, [gt.b(), BD.b()], [pb.b()])
                k.tt("dve", X[:, tile, cs], X[:, tile, cs], pb[:, :], ALU.add, [X.b(tile), pb.b()], [X.b(tile)])
            layernorm_tile(k, X, tile, G2, B2, stt_)
            if last:
                fw.dma("sp", dr["y"][tile * 128:(tile + 1) * 128, :], X[:, tile, :], reads=[X.b(tile)], final=True)
            else:
                make_xT(k, tile, layer + 1, False)
        fw.flush()


def layernorm_tile(k, X, tile, Gt, Bt, stt_):
    fw = k.fw
    xs = X[:, tile, :]
    for h in range(2):
        fw.op("dve", lambda e, h=h: e.bn_stats(out=stt_[:, h * 6:(h + 1) * 6], in_=X[:, tile, h * 512:(h + 1) * 512]),
              [X.b(tile)], [stt_.b()])
    fw.op("dve", lambda e: e.bn_aggr(out=stt_[:, 12:14], in_=stt_[:, 0:12]), [stt_.b()], [stt_.b()])
    k.act(stt_[:, 14:15], stt_[:, 13:14], AF.Sqrt, [stt_.b()], [stt_.b()], bias=k.C_eps[:, 0:1])
    fw.op("dve", lambda e: e.reciprocal(out=stt_[:, 15:16], in_=stt_[:, 14:15]), [stt_.b()], [stt_.b()])
    k.ts("dve", xs, xs, stt_[:, 12:13], stt_[:, 15:16], ALU.subtract, ALU.mult, [X.b(tile), stt_.b()], [X.b(tile)])
    k.tt("pool", xs, xs, Gt[:], ALU.mult, [X.b(tile), Gt.b()], [X.b(tile)])
    k.tt("pool", xs, xs, Bt[:], ALU.add, [X.b(tile), Bt.b()], [X.b(tile)])


def load_layer_router(k, layer):
    dr = k.dram
    k.fw.dma("sp", k.B_wrt[:], dr["w_rt"][layer * D:(layer + 1) * D, :].rearrange("(c p) e -> p c e", p=128), writes=[k.B_wrt.b()])
    k.fw.dma("sp", k.B_brt[:], dr["b_rt"][layer:layer + 1, :].partition_broadcast(128), writes=[k.B_brt.b()])


def build(cfg):
    k = K(cfg)
    declare_dram(k)
    if cfg.get("dbg"):
        for n, shp in cfg["dbg"].items():
            k.dout(n, shp)
    with k.st:
        k.setup()
        fw = k.fw
        k.X = k.sb("X", [128, NT, D])
        k.XT = k.sb("XT", [128, 8, SEQ], BF16)
        k.B_xtf = k.sb("xtf", [128, 8, 128])
        k.B_wrt = k.sb("wrt", [128, 8, NEXP])
        k.B_brt = k.sb("brt", [128, NEXP])
        k.B_lg = k.sb("lg", [128, NT, NEXP])
        k.C_eps = k.sb("eps", [128, 4])
        k.vf = k.nc.dram_tensor("vfirst", [W, SEQ], F32, kind="Internal").ap()
        k.vfb = Buf()
        load_consts(k)
        fw.op("dve", lambda e: e.memset(k.C_eps[:, 0:1], LN_EPS), [], [k.C_eps.b()])
        fw.op("dve", lambda e: e.memset(k.C_eps[:, 1:2], NORM_EPS), [], [k.C_eps.b()])
        for tile in range(NT):
            fw.dma("sp", k.X[:, tile, :], k.dram["x"][tile * 128:(tile + 1) * 128, :], writes=[k.X.b(tile)])
        layers = cfg.get("layers", list(range(DEPTH)))
        phases = cfg.get("phases", "AB")
        stop = cfg.get("stop", "")

        def dump():
            for tile in range(NT):
                fw.dma("sp", k.dram["y"][tile * 128:(tile + 1) * 128, :], k.X[:, tile, :], reads=[k.X.b(tile)], final=True)
            fw.flush()
        if stop == "xload":
            dump()
            return k
        if "A" not in phases:
            load_layer_router(k, layers[0])
        for tile in range(cfg.get("nxt", NT)):
            make_xT(k, tile, layers[0], "A" not in phases and stop != "xT0")
        fw.flush()
        if stop.startswith("xT"):
            dump()
            return k
        for li, layer in enumerate(layers):
            last = li == len(layers) - 1
            if "A" in phases:
                phase_a(k, layer)
            if "B" in phases:
                phase_b(k, layer, last)
            else:
                for tile in range(NT):
                    fw.dma("sp", k.dram["y"][tile * 128:(tile + 1) * 128, :], k.X[:, tile, :], reads=[k.X.b(tile)], final=True)
                fw.flush()
    return k


_CACHE = {}


def kernel(**inputs):
    maps = build_inmaps(inputs)
    if "k" not in _CACHE:
        _CACHE["k"] = build({})
    k = _CACHE["k"]
    res = run_bass_kernel_spmd(k.nc, maps, core_ids=list(range(8)))
    out = np.stack([np.asarray(res.results[b]["y"], np.float32) for b in range(8)], axis=0)
    return out


COLP = {}


def _colp_layout():
    off = 0
    spec = [("mu", 14), ("w0", 4), ("a0", 4), ("v0", 4), ("k_k", 4), ("k_a", 4), ("r_k", 4), ("lnx_g", 4), ("lnx_b", 4),
            ("hg_nw", 4), ("ml_cw", 32), ("ml_cb", 8), ("ml_nw", 4), ("lr_cw", 16), ("lr_cb", 4), ("lr_bx", 4), ("lr_ba", 4),
            ("lr_lam", 4), ("hg_lb", 16)]
    for n, c in spec:
        COLP[n] = (off, c)
        off += c
    return off


NCOLP = _colp_layout()
OFF_B = 1792
OFF_C = 3840
OFF_D = 5896
OFF_G = 6920


def host_colp(inputs):
    L = DEPTH
    out = np.zeros((L, 128, NCOLP), np.float32)
    g = lambda n: np.asarray(inputs[n], np.float32)

    def put(l, name, arr2d):
        o, c = COLP[name]
        assert arr2d.shape == (128, c), (name, arr2d.shape)
        out[l, :, o:o + c] = arr2d
    for l in range(L):
        put(l, "mu", _cols(g("rwkv_mu")[l]))
        put(l, "w0", _cols(g("rwkv_w0")[l])); put(l, "a0", _cols(g("rwkv_a0")[l]))
        if l > 0:
            put(l, "v0", _cols(g("rwkv_v0")[l - 1]))
        put(l, "k_k", _cols(g("rwkv_k_k")[l])); put(l, "k_a", _cols(g("rwkv_k_a")[l]))
        put(l, "r_k", _cols(g("rwkv_r_k")[l].reshape(-1)))
        put(l, "lnx_g", _cols(g("rwkv_lnx_g")[l])); put(l, "lnx_b", _cols(g("rwkv_lnx_b")[l]))
        put(l, "hg_nw", _cols(g("hgrn_norm_w")[l]))
        put(l, "ml_cw", np.concatenate([_cols(g("mlstm_conv_w")[l, j]) for j in range(4)], axis=1))
        put(l, "ml_cb", _cols(g("mlstm_conv_b")[l])); put(l, "ml_nw", _cols(g("mlstm_norm_w")[l]))
        put(l, "lr_cw", np.concatenate([_cols(g("lru_conv_w")[l, j]) for j in range(4)], axis=1))
        put(l, "lr_cb", _cols(g("lru_conv_b")[l]))
        put(l, "lr_bx", _cols(g("lru_bx")[l].reshape(-1))); put(l, "lr_ba", _cols(g("lru_ba")[l].reshape(-1)))
        put(l, "lr_lam", _cols(g("lru_lambda")[l]))
        put(l, "hg_lb", np.concatenate([_cols(g("hgrn_lower_bounds")[ll]) for ll in range(L)], axis=1))
    return out.reshape(L * 128, NCOLP)


def build_inmaps_a(inputs, shared):
    f = lambda a: np.ascontiguousarray(np.asarray(a, np.float32))
    L = DEPTH
    shared["colp"] = host_colp(inputs)
    shared["rw_w2"] = f(inputs["rwkv_w2"]).reshape(L * 64, W)
    shared["rw_a2"] = f(inputs["rwkv_a2"]).reshape(L * 64, W)
    shared["rw_v2"] = f(inputs["rwkv_v2"]).reshape(3 * 32, W)
    shared["rw_g2"] = f(inputs["rwkv_g2"]).reshape(L * 128, W)
    shared["lr_wx"] = f(inputs["lru_wx"]).reshape(L * 8 * 64, 64)
    shared["lr_wa"] = f(inputs["lru_wa"]).reshape(L * 8 * 64, 64)
    shared["ml_ib"] = f(inputs["mlstm_i_bias"])
    shared["ml_fb"] = f(inputs["mlstm_f_bias"])


def declare_dram_a(k):
    L = DEPTH
    k.din("colp", [L * 128, NCOLP])
    k.din("rw_w2", [L * 64, W]); k.din("rw_a2", [L * 64, W]); k.din("rw_v2", [3 * 32, W]); k.din("rw_g2", [L * 128, W])
    k.din("lr_wx", [L * 8 * 64, 64]); k.din("lr_wa", [L * 8 * 64, 64])
    k.din("ml_ib", [L, 4]); k.din("ml_fb", [L, 4])


def w_in_ap(k, layer, c0, nc_):
    if layer == 0:
        return k.dram["w_in_first"][:, c0:c0 + nc_]
    return k.dram["w_in_rest"][(layer - 1) * D:layer * D, c0:c0 + nc_]


def load_w_multi(k, pieces, kc=8):
    tot = sum(int(p.shape[1]) for p in pieces)
    s = wslot(k)
    full = k.ring[s]
    view = full[:, 0:kc * tot].rearrange("p (a c) -> p a c", a=kc)
    o = 0
    for p in pieces:
        n = int(p.shape[1])
        k.fw.dma("pool", view[:, :, o:o + n], p.rearrange("(a p) c -> p a c", p=128), writes=[k.ringb[s]])
        o += n
    return view, k.ringb[s]


def zt_mm(k, wview, wbuf, c0, ncols, tb, M=None):
    p = k.ps()
    ts_ = slice(tb * TBA, (tb + 1) * TBA)
    xb = [k.XT.b(tb * 2 + i) for i in range(2)]
    for c in range(8):
        k.mm(p[0:ncols, 0:TBA], wview[:, c, c0:c0 + ncols], k.XT[:, c, ts_], c == 0, c == 7, xb + [wbuf], [p.b()])
    return p


def cp_(k, name, j=0):
    o, c = COLP[name]
    return k.A["colp"][:, o + j:o + j + 1]


def phase_a(k, layer):
    fw = k.fw
    nc = k.nc
    dr = k.dram
    st = contextlib.ExitStack()
    mixers = k.cfg.get("mixers", "ABCD")
    with st:
        A = {}
        k.A = A
        k.NRING = 3
        k.ring = []
        k.ringb = []
        for i in range(k.NRING):
            k.ring.append(st.enter_context(nc.sbuf_tensor("ringA%d_%d" % (layer, i), [128, 4096], BF16)))
            k.ringb.append(Buf())
        k.ring_rr = 0
        A["colp"] = k.sb("colp%d" % layer, [128, NCOLP], F32, st)
        fw.dma("sp", A["colp"][:], dr["colp"][layer * 128:(layer + 1) * 128, :], writes=[A["colp"].b()])
        A["YT"] = [k.sb("YT%d_%d" % (layer, n), [128, 4, TBA], BF16, st) for n in range(4)]
        A["MT"] = k.sb("MT%d" % layer, [128, 8, TBA], BF16, st)
        A["G1"] = k.sb("G1_%d" % layer, [128, D], F32, st)
        A["B1"] = k.sb("B1_%d" % layer, [128, D], F32, st)
        fw.dma("sp", A["G1"][:], dr["ln1_g"][layer:layer + 1, :].partition_broadcast(128), writes=[A["G1"].b()])
        fw.dma("sp", A["B1"][:], dr["ln1_b"][layer:layer + 1, :].partition_broadcast(128), writes=[A["B1"].b()])
        NS = k.cfg.get("NS", 16)
        A["S"] = [k.sb("S%d_%d" % (layer, i), [128, 520], F32, st) for i in range(NS)]
        A["sm"] = k.sb("smA%d" % layer, [128, 64], F32, st)
        A["stt"] = k.sb("sttA%d" % layer, [128, 16], F32, st)
        load_layer_router(k, layer)
        for n in range(4):
            if "ABCD"[n] not in mixers:
                fw.op("pool", lambda e, n=n: e.memset(A["YT"][n][:], 0.0), [], [A["YT"][n].b()])
        if "D" in mixers:
            lru_setup(k, layer, st)
        if "C" in mixers:
            mlstm_setup(k, layer, st)
        if "B" in mixers:
            hgrn_setup(k, layer, st)
        if "A" in mixers:
            rwkv_setup(k, layer, st)
        for tb in range(k.cfg.get("ntb", NBA)):
            if "D" in mixers:
                lru_block(k, layer, tb)
            if "C" in mixers:
                mlstm_block(k, layer, tb)
            if "B" in mixers:
                hgrn_block(k, layer, tb)
            if "A" in mixers:
                rwkv_block(k, layer, tb)
            if k.cfg.get("dbgY"):
                for n in range(4):
                    for j in range(4):
                        fw.dma("pool", dr["dbg_y"][n * W + j * 128:n * W + (j + 1) * 128, tb * TBA:(tb + 1) * TBA],
                               A["YT"][n][:, j, :], reads=[A["YT"][n].b()], final=True)
            if not k.cfg.get("nomerge"):
                merge_block(k, layer, tb)
        fw.flush()


def lru_setup(k, layer, st):
    fw = k.fw
    A = k.A
    dr = k.dram
    A["lr_wx"] = k.sb("lrwx%d" % layer, [128, 4, 128], F32, st)
    A["lr_wa"] = k.sb("lrwa%d" % layer, [128, 4, 128], F32, st)
    for nm, src in (("lr_wx", "lr_wx"), ("lr_wa", "lr_wa")):
        t = A[nm]
        fw.op("dve", lambda e, t=t: e.memset(t[:], 0.0), [], [t.b()])
        for g in range(8):
            j, h = g // 2, g % 2
            r0 = (layer * 8 + g) * 64
            fw.dma("sp", t[h * 64:(h + 1) * 64, j, h * 64:(h + 1) * 64], dr[src][r0:r0 + 64, :], writes=[t.b()])
    A["lr_c8"] = k.sb("lrc8_%d" % layer, [128, 8], F32, st)
    c8 = A["lr_c8"]
    o, _ = COLP["lr_lam"]
    k.act(c8[:, 0:4], A["colp"][:, o:o + 4], AF.Sigmoid, [A["colp"].b()], [c8.b()])
    k.act(c8[:, 0:4], c8[:, 0:4], AF.Ln, [c8.b()], [c8.b()])
    k.ts("dve", c8[:, 4:8], c8[:, 0:4], 16.0, None, ALU.mult, None, [c8.b()], [c8.b()])
    k.ts("dve", c8[:, 0:4], c8[:, 0:4], 8.0, None, ALU.mult, None, [c8.b()], [c8.b()])
    A["lr_halo"] = k.sb("lrhalo%d" % layer, [128, 4, 4], F32, st)
    A["lr_hc"] = k.sb("lrhc%d" % layer, [128, 4], F32, st)
    fw.op("dve", lambda e: e.memset(A["lr_halo"][:], 0.0), [], [A["lr_halo"].b()])
    fw.op("dve", lambda e: e.memset(A["lr_hc"][:], 0.0), [], [A["lr_hc"].b()])


def conv4(k, zt, acc, wname, bname, j, nchunks):
    o, _ = COLP[wname]
    cw = lambda i: k.A["colp"][:, o + i * nchunks + j:o + i * nchunks + j + 1]
    cb = cp_(k, bname, j)
    rb = [zt.b(), k.A["colp"].b()]
    k.ts("dve", acc[:, 0:TBA], zt[:, 3:3 + TBA], cw(3), cb, ALU.mult, ALU.add, rb, [acc.b()])
    for i in (2, 1, 0):
        k.stt(acc[:, 0:TBA], zt[:, i:i + TBA], cw(i), acc[:, 0:TBA], ALU.mult, ALU.add, rb + [acc.b()], [acc.b()])


def lru_block(k, layer, tb):
    fw = k.fw
    A = k.A
    S = A["S"]
    YT = A["YT"][3]
    wx, bx_ = load_w(k, w_in_ap(k, layer, OFF_D, 512), 8, 512)
    wg, bg_ = load_w(k, w_in_ap(k, layer, OFF_D + 512, 512), 8, 512)
    halo, hc, c8 = A["lr_halo"], A["lr_hc"], A["lr_c8"]
    for j in range(4):
        zt, xc, ga, gx, t1, u = S[0], S[1], S[2], S[3], S[4], S[5]
        p = zt_mm(k, wx, bx_, j * 128, 128, tb)
        k.cp("dve", zt[:, 0:3], halo[:, j, 0:3], [halo.b()], [zt.b()])
        k.cp("act", zt[:, 3:3 + TBA], p[:, 0:TBA], [p.b()], [zt.b()])
        k.cp("dve", halo[:, j, 0:3], zt[:, TBA:TBA + 3], [zt.b()], [halo.b()])
        conv4(k, zt, xc, "lr_cw", "lr_cb", j, 4)
        pa = k.ps()
        k.mm(pa[:, 0:TBA], A["lr_wa"][:, j, :], xc[:, 0:TBA], True, True, [A["lr_wa"].b(), xc.b()], [pa.b()])
        px = k.ps()
        k.mm(px[:, 0:TBA], A["lr_wx"][:, j, :], xc[:, 0:TBA], True, True, [A["lr_wx"].b(), xc.b()], [px.b()])
        k.act(ga[:, 0:TBA], pa[:, 0:TBA], AF.Sigmoid, [pa.b(), A["colp"].b()], [ga.b()], bias=cp_(k, "lr_ba", j))
        k.act(gx[:, 0:TBA], px[:, 0:TBA], AF.Sigmoid, [px.b(), A["colp"].b()], [gx.b()], bias=cp_(k, "lr_bx", j))
        k.ts("dve", ga[:, 0:TBA], ga[:, 0:TBA], c8[:, j:j + 1], None, ALU.mult, None, [ga.b(), c8.b()], [ga.b()])
        k.act(ga[:, 0:TBA], ga[:, 0:TBA], AF.Exp, [ga.b()], [ga.b()])
        k.tt("dve", t1[:, 0:TBA], ga[:, 0:TBA], ga[:, 0:TBA], ALU.mult, [ga.b()], [t1.b()])
        k.ts("dve", t1[:, 0:TBA], t1[:, 0:TBA], -1.0, 1.0, ALU.mult, ALU.add, [t1.b()], [t1.b()])
        k.act(t1[:, 0:TBA], t1[:, 0:TBA], AF.Sqrt, [t1.b()], [t1.b()])
        if tb == 0:
            fw.op("dve", lambda e, t1=t1: e.memset(t1[:, 0:1], 1.0), [], [t1.b()])
        k.tt("dve", gx[:, 0:TBA], gx[:, 0:TBA], xc[:, 0:TBA], ALU.mult, [gx.b(), xc.b()], [gx.b()])
        k.tt("dve", gx[:, 0:TBA], gx[:, 0:TBA], t1[:, 0:TBA], ALU.mult, [gx.b(), t1.b()], [gx.b()])
        fw.op("dve", lambda e, ga=ga, gx=gx, xc=xc, j=j: e.tensor_tensor_scan(
            out=xc[:, 0:TBA], data0=ga[:, 0:TBA], data1=gx[:, 0:TBA], initial=hc[:, j:j + 1], op0=ALU.mult, op1=ALU.add),
            [ga.b(), gx.b(), hc.b()], [xc.b()])
        k.cp("dve", hc[:, j:j + 1], xc[:, TBA - 1:TBA], [xc.b()], [hc.b()])
        pg = zt_mm(k, wg, bg_, j * 128, 128, tb)
        k.cp("act", u[:, 0:TBA], pg[:, 0:TBA], [pg.b()], [u.b()])
        k.tt("dve", t1[:, 0:TBA], u[:, 0:TBA], u[:, 0:TBA], ALU.mult, [u.b()], [t1.b()])
        k.ts("dve", t1[:, 0:TBA], t1[:, 0:TBA], 0.044715, 1.0, ALU.mult, ALU.add, [t1.b()], [t1.b()])
        k.tt("dve", t1[:, 0:TBA], t1[:, 0:TBA], u[:, 0:TBA], ALU.mult, [t1.b(), u.b()], [t1.b()])
        k.act(t1[:, 0:TBA], t1[:, 0:TBA], AF.Sigmoid, [t1.b()], [t1.b()], scale=1.5957691216057308)
        k.tt("dve", t1[:, 0:TBA], t1[:, 0:TBA], u[:, 0:TBA], ALU.mult, [t1.b(), u.b()], [t1.b()])
        k.tt("dve", YT[:, j, :], t1[:, 0:TBA], xc[:, 0:TBA], ALU.mult, [t1.b(), xc.b()], [YT.b()])


def merge_block(k, layer, tb):
    fw = k.fw
    A = k.A
    S = A["S"]
    dr = k.dram
    MT = A["MT"]
    X = k.X
    for c in range(8):
        wg, bg_ = load_w_multi(k, [w_in_ap(k, layer, OFF_G + n * D + c * 128, 128) for n in range(4)], 8)
        wb, bb_ = load_w_multi(k, [dr["w_br"][(layer * 4 + n) * W:(layer * 4 + n + 1) * W, c * 128:(c + 1) * 128] for n in range(4)], 4)
        acc = S[0]
        for n in range(4):
            pz = zt_mm(k, wg, bg_, n * 128, 128, tb)
            sg = S[1 + (n % 2)]
            k.act(sg[:, 0:TBA], pz[:, 0:TBA], AF.Sigmoid, [pz.b()], [sg.b()])
            pp = k.ps()
            for kc in range(4):
                k.mm(pp[:, 0:TBA], wb[:, kc, n * 128:(n + 1) * 128], A["YT"][n][:, kc, :], kc == 0, kc == 3,
                     [bb_, A["YT"][n].b()], [pp.b()])
            if n == 0:
                k.tt("dve", acc[:, 0:TBA], sg[:, 0:TBA], pp[:, 0:TBA], ALU.mult, [sg.b(), pp.b()], [acc.b()])
            else:
                k.tt("dve", sg[:, 0:TBA], sg[:, 0:TBA], pp[:, 0:TBA], ALU.mult, [sg.b(), pp.b()], [sg.b()])
                if n < 3:
                    k.tt("pool", acc[:, 0:TBA], acc[:, 0:TBA], sg[:, 0:TBA], ALU.add, [acc.b(), sg.b()], [acc.b()])
                else:
                    k.tt("pool", MT[:, c, :], acc[:, 0:TBA], sg[:, 0:TBA], ALU.add, [acc.b(), sg.b()], [MT.b()])
    wo = [load_w(k, dr["w_out"][layer * D:(layer + 1) * D, h * 512:(h + 1) * 512], 8, 512) for h in range(2)]
    for i in range(TBA // 128):
        tile = tb * (TBA // 128) + i
        for h in range(2):
            cs = slice(h * 512, (h + 1) * 512)
            p = k.ps()
            for kc in range(8):
                k.mm(p[:, :], MT[:, kc, i * 128:(i + 1) * 128], wo[h][0][:, kc, :], kc == 0, kc == 7, [MT.b(), wo[h][1]], [p.b()])
            k.stt(X[:, tile, cs], X[:, tile, cs], ALPHA, p[:, :], ALU.mult, ALU.add, [X.b(tile), p.b()], [X.b(tile)])
        layernorm_tile(k, X, tile, A["G1"], A["B1"], A["stt"])
        make_xT(k, tile, layer, True)


def v3(ap2d, c):
    return ap2d.rearrange("p (c t) -> p c t", c=c)


def hgrn_setup(k, layer, st):
    fw = k.fw
    A = k.A
    A["hg_S"] = [k.sb("hgS%d_%d" % (layer, h), [128, 128], F32, st) for h in range(4)]
    for h in range(4):
        fw.op("pool", lambda e, h=h: e.memset(A["hg_S"][h][:], 0.0), [], [A["hg_S"][h].b()])
    lbt = k.sb("hglb%d" % layer, [128, 32], F32, st)
    A["hg_lbt"] = lbt
    o, _ = COLP["hg_lb"]
    rb = [A["colp"].b()]
    E = lbt[:, 0:16]
    k.act(E, A["colp"][:, o:o + 16], AF.Exp, rb, [lbt.b()])
    tot, num = lbt[:, 16:20], lbt[:, 20:24]
    k.tt("dve", tot, lbt[:, 0:4], lbt[:, 4:8], ALU.add, [lbt.b()], [lbt.b()])
    k.tt("dve", tot, tot, lbt[:, 8:12], ALU.add, [lbt.b()], [lbt.b()])
    k.tt("dve", tot, tot, lbt[:, 12:16], ALU.add, [lbt.b()], [lbt.b()])
    fw.op("dve", lambda e: e.memset(num, 0.0), [], [lbt.b()])
    for ll in range(1, layer + 1):
        k.tt("dve", num, num, lbt[:, ll * 4:(ll + 1) * 4], ALU.add, [lbt.b()], [lbt.b()])
    fw.op("dve", lambda e: e.reciprocal(out=tot, in_=tot), [lbt.b()], [lbt.b()])
    k.tt("dve", lbt[:, 24:28], num, tot, ALU.mult, [lbt.b()], [lbt.b()])
    k.ts("dve", lbt[:, 28:32], lbt[:, 24:28], -1.0, 1.0, ALU.mult, ALU.add, [lbt.b()], [lbt.b()])


def hgrn_block(k, layer, tb):
    fw = k.fw
    A = k.A
    S = A["S"]
    C = k.C
    sm = A["sm"]
    lbt = A["hg_lbt"]
    YT = A["YT"][1]
    NCH = TBA // 64
    ident = C["ident"]
    for h in range(4):
        wv, wb_ = load_w_multi(k, [w_in_ap(k, layer, OFF_B + q * W + h * 128, 128) for q in range(4)], 8)
        qt, bc, kt, eq, ek, vT, sgt, Kt, Vt, AT, O, sq, Sp = S[0:13]
        St = A["hg_S"][h]
        pq = zt_mm(k, wv, wb_, 0, 128, tb)
        k.act(qt[:, 0:TBA], pq[:, 0:TBA], AF.Silu, [pq.b()], [qt.b()])
        pf = zt_mm(k, wv, wb_, 128, 128, tb)
        k.act(kt[:, 0:TBA], pf[:, 0:TBA], AF.Sigmoid, [pf.b()], [kt.b()])
        k.ts("dve", kt[:, 0:TBA], kt[:, 0:TBA], lbt[:, 28 + h:29 + h], lbt[:, 24 + h:25 + h], ALU.mult, ALU.add, [kt.b(), lbt.b()], [kt.b()])
        k.act(bc[:, 0:TBA], kt[:, 0:TBA], AF.Ln, [kt.b()], [bc.b()])
        k.ts("dve", kt[:, 0:TBA], kt[:, 0:TBA], -1.0, 1.0, ALU.mult, ALU.add, [kt.b()], [kt.b()])
        fw.op("dve", lambda e, bc=bc: e.tensor_tensor_scan(out=bc[:, 0:TBA], data0=C["rmask"][:, 0:TBA], data1=bc[:, 0:TBA],
                                                            initial=0.0, op0=ALU.mult, op1=ALU.add), [bc.b(), C["rmask"].b()], [bc.b()])
        bc3 = v3(bc[:, 0:TBA], NCH)
        k.act(sm[:, 0:NCH], bc3[:, :, 63], AF.Exp, [bc.b()], [sm.b()])
        k.act(sm[:, 8:8 + NCH], bc3[:, :, 31], AF.Exp, [bc.b()], [sm.b()])
        k.cp("dve", sm[:, 16:16 + NCH], bc3[:, :, 31], [bc.b()], [sm.b()])
        k.tt("dve", bc3, bc3, v3(sm[:, 16:16 + NCH], NCH).broadcast_to([128, NCH, 64]), ALU.subtract, [bc.b(), sm.b()], [bc.b()])
        k.act(sm[:, 4:4 + NCH], bc3[:, :, 63], AF.Exp, [bc.b()], [sm.b()])
        k.act(eq[:, 0:TBA], bc[:, 0:TBA], AF.Exp, [bc.b()], [eq.b()])
        k.act(ek[:, 0:TBA], bc[:, 0:TBA], AF.Exp, [bc.b()], [ek.b()], scale=-1.0)
        k.tt("dve", qt[:, 0:TBA], qt[:, 0:TBA], eq[:, 0:TBA], ALU.mult, [qt.b(), eq.b()], [qt.b()])
        k.tt("dve", kt[:, 0:TBA], kt[:, 0:TBA], ek[:, 0:TBA], ALU.mult, [kt.b(), ek.b()], [kt.b()])
        pv = zt_mm(k, wv, wb_, 256, 128, tb)
        k.cp("act", vT[:, 0:TBA], pv[:, 0:TBA], [pv.b()], [vT.b()])
        pg = zt_mm(k, wv, wb_, 384, 128, tb)
        k.act(sgt[:, 0:TBA], pg[:, 0:TBA], AF.Silu, [pg.b()], [sgt.b()])
        for src, dst in ((kt, Kt), (vT, Vt)):
            p = k.ps()
            for c in range(NCH):
                k.tr(p[0:64, c * 128:(c + 1) * 128], src[:, c * 64:(c + 1) * 64], ident[:], [src.b(), ident.b()], [p.b()])
            k.cp("act", dst[0:64, 0:NCH * 128], p[0:64, 0:NCH * 128], [p.b()], [dst.b()])
        pA = k.ps()
        for c in range(NCH):
            cs = slice(c * 64, (c + 1) * 64)
            k.mm(pA[0:64, cs], kt[:, cs], qt[:, cs], True, True, [kt.b(), qt.b()], [pA.b()])
        k.tt("dve", AT[0:64, 0:TBA], pA[0:64, 0:TBA], C["m_incl"][0:64, 0:TBA], ALU.mult, [pA.b(), C["m_incl"].b()], [AT.b()])
        pKV = k.ps()
        for c in range(NCH):
            k.mm(pKV[:, c * 128:(c + 1) * 128], Kt[0:64, c * 128:(c + 1) * 128], Vt[0:64, c * 128:(c + 1) * 128], True, True,
                 [Kt.b(), Vt.b()], [pKV.b()])
        for c in range(NCH):
            cs = slice(c * 64, (c + 1) * 64)
            k.ts("dve", Sp[:, 0:128], St[:], sm[:, 8 + c:9 + c], None, ALU.mult, None, [St.b(), sm.b()], [Sp.b()])
            po = k.ps()
            k.mm(po[0:64, 0:128], qt[:, cs], Sp[:, 0:128], True, False, [qt.b(), Sp.b()], [po.b()])
            k.mm(po[0:64, 0:128], AT[0:64, cs], Vt[0:64, c * 128:(c + 1) * 128], False, True, [AT.b(), Vt.b()], [po.b()])
            k.cp("act", O[0:64, c * 128:(c + 1) * 128], po[0:64, 0:128], [po.b()], [O.b()])
            k.ts("dve", St[:], St[:], sm[:, c:c + 1], None, ALU.mult, None, [St.b(), sm.b()], [St.b()])
            k.stt(St[:], pKV[:, c * 128:(c + 1) * 128], sm[:, 4 + c:5 + c], St[:], ALU.mult, ALU.add, [pKV.b(), sm.b(), St.b()], [St.b()])
        O3 = v3(O[0:64, 0:NCH * 128], NCH)
        k.tt("dve", sq[0:64, 0:NCH * 128], O[0:64, 0:NCH * 128], O[0:64, 0:NCH * 128], ALU.mult, [O.b()], [sq.b()])
        fw.op("dve", lambda e, sq=sq: e.tensor_reduce(out=sm[0:64, 24:24 + NCH], in_=v3(sq[0:64, 0:NCH * 128], NCH), axis=AX.X, op=ALU.add),
              [sq.b()], [sm.b()])
        k.ts("dve", sm[0:64, 24:24 + NCH], sm[0:64, 24:24 + NCH], 1.0 / 128, NORM_EPS, ALU.mult, ALU.add, [sm.b()], [sm.b()])
        k.act(sm[0:64, 24:24 + NCH], sm[0:64, 24:24 + NCH], AF.Sqrt, [sm.b()], [sm.b()])
        fw.op("dve", lambda e: e.reciprocal(out=sm[0:64, 28:28 + NCH], in_=sm[0:64, 24:24 + NCH]), [sm.b()], [sm.b()])
        k.tt("dve", O3, O3, v3(sm[0:64, 28:28 + NCH], NCH).broadcast_to([64, NCH, 128]), ALU.mult, [O.b(), sm.b()], [O.b()])
        pT = k.ps()
        for c in range(NCH):
            k.tr(pT[:, c * 64:(c + 1) * 64], O[0:64, c * 128:(c + 1) * 128], ident[0:64, 0:64], [O.b(), ident.b()], [pT.b()])
        k.stt(YT[:, h, :], pT[:, 0:TBA], cp_(k, "hg_nw", h), sgt[:, 0:TBA], ALU.mult, ALU.mult, [pT.b(), A["colp"].b(), sgt.b()], [YT.b()])


def mlstm_setup(k, layer, st):
    fw = k.fw
    A = k.A
    dr = k.dram
    A["ml_C"] = [k.sb("mlC%d_%d" % (layer, h), [128, 132], F32, st) for h in range(4)]
    for h in range(4):
        fw.op("pool", lambda e, h=h: e.memset(A["ml_C"][h][:], 0.0), [], [A["ml_C"][h].b()])
    A["ml_halo"] = k.sb("mlhalo%d" % layer, [128, 8, 4], F32, st)
    fw.op("pool", lambda e: e.memset(A["ml_halo"][:], 0.0), [], [A["ml_halo"].b()])
    A["ml_g"] = k.sb("mlg%d" % layer, [64, 112], F32, st)
    A["ml_efl"] = k.sb("mlefl%d" % layer, [128, 16], F32, st)
    g = A["ml_g"]
    fw.dma("sp", g[:, 96:100], dr["ml_ib"][layer:layer + 1, :].partition_broadcast(64), writes=[g.b()])
    fw.dma("sp", g[:, 100:104], dr["ml_fb"][layer:layer + 1, :].partition_broadcast(64), writes=[g.b()])


def mlstm_block(k, layer, tb):
    fw = k.fw
    A = k.A
    S = A["S"]
    C = k.C
    sm = A["sm"]
    YT = A["YT"][2]
    NCH = TBA // 64
    ident = C["ident"]
    g = A["ml_g"]
    efl = A["ml_efl"]
    XT = k.XT
    wgv, wgb = load_w(k, w_in_ap(k, layer, OFF_C + 2048, 8), 8, 8)
    pgt = k.ps()
    for c in range(NCH):
        t0 = tb * TBA + c * 64
        tl = t0 // 128
        for kc in range(8):
            k.mm(pgt[0:64, c * 8:(c + 1) * 8], XT[:, kc, t0:t0 + 64], wgv[:, kc, :], kc == 0, kc == 7, [XT.b(tl), wgb], [pgt.b()])
    GT3 = v3(g[:, 0:32], NCH)
    k.tt("dve", GT3, v3(pgt[0:64, 0:32], NCH), g[:, 96:104].unsqueeze(1).broadcast_to([64, NCH, 8]), ALU.add, [pgt.b(), g.b()], [g.b()])
    LF3 = v3(g[:, 32:48], NCH)
    k.act(LF3, GT3[:, :, 4:8], AF.Sigmoid, [g.b()], [g.b()])
    k.act(g[:, 32:48], g[:, 32:48], AF.Ln, [g.b()], [g.b()])
    pF = k.ps()
    k.mm(pF[0:64, 0:16], C["m_incl"][0:64, 0:64], g[:, 32:48], True, True, [C["m_incl"].b(), g.b()], [pF.b()])
    pL = k.ps()
    k.mm(pL[:, 0:16], C["ones"][0:64, :], g[:, 32:48], True, True, [C["ones"].b(), g.b()], [pL.b()])
    k.act(efl[:, 0:16], pL[:, 0:16], AF.Exp, [pL.b()], [efl.b()])
    k.tt("dve", v3(g[:, 64:80], NCH), GT3[:, :, 0:4], v3(pF[0:64, 0:16], NCH), ALU.subtract, [g.b(), pF.b()], [g.b()])
    k.act(g[:, 64:80], g[:, 64:80], AF.Exp, [g.b()], [g.b()])
    k.act(g[:, 80:96], pF[0:64, 0:16], AF.Exp, [pF.b()], [g.b()])
    halo = A["ml_halo"]
    for h in range(4):
        wv, wb_ = load_w_multi(k, [w_in_ap(k, layer, OFF_C + q * W + h * 128, 128) for q in range(4)], 8)
        zt, qT, kT, vT, so, Kg, Vx, WT, NUM, sq = S[0:10]
        Ct = A["ml_C"][h]
        for qi, dst in ((0, qT), (1, kT)):
            hj = qi * 4 + h
            p = zt_mm(k, wv, wb_, qi * 128, 128, tb)
            k.cp("dve", zt[:, 0:3], halo[:, hj, 0:3], [halo.b()], [zt.b()])
            k.cp("act", zt[:, 3:3 + TBA], p[:, 0:TBA], [p.b()], [zt.b()])
            k.cp("dve", halo[:, hj, 0:3], zt[:, TBA:TBA + 3], [zt.b()], [halo.b()])
            conv4(k, zt, dst, "ml_cw", "ml_cb", hj, 8)
            k.act(dst[:, 0:TBA], dst[:, 0:TBA], AF.Silu, [dst.b()], [dst.b()])
        k.ts("dve", kT[:, 0:TBA], kT[:, 0:TBA], float(128 ** -0.5), None, ALU.mult, None, [kT.b()], [kT.b()])
        pv = zt_mm(k, wv, wb_, 256, 128, tb)
        k.cp("act", vT[:, 0:TBA], pv[:, 0:TBA], [pv.b()], [vT.b()])
        po_ = zt_mm(k, wv, wb_, 384, 128, tb)
        k.act(so[:, 0:TBA], po_[:, 0:TBA], AF.Sigmoid, [po_.b()], [so.b()])
        p = k.ps()
        for c in range(NCH):
            k.tr(p[0:64, c * 128:(c + 1) * 128], kT[:, c * 64:(c + 1) * 64], ident[:], [kT.b(), ident.b()], [p.b()])
        for c in range(NCH):
            k.ts("dve", Kg[0:64, c * 128:(c + 1) * 128], p[0:64, c * 128:(c + 1) * 128], g[:, 64 + c * 4 + h:65 + c * 4 + h], None,
                 ALU.mult, None, [p.b(), g.b()], [Kg.b()])
        p = k.ps()
        for c in range(NCH):
            k.tr(p[0:64, c * 128:(c + 1) * 128], vT[:, c * 64:(c + 1) * 64], ident[:], [vT.b(), ident.b()], [p.b()])
        Vx3 = v3(Vx[0:64, 0:NCH * 129], NCH)
        k.cp("act", Vx3[:, :, 0:128], v3(p[0:64, 0:NCH * 128], NCH), [p.b()], [Vx.b()])
        fw.op("dve", lambda e, Vx3=Vx3: e.memset(Vx3[:, :, 128:129], 1.0), [], [Vx.b()])
        pA = k.ps()
        for c in range(NCH):
            cs = slice(c * 64, (c + 1) * 64)
            k.mm(pA[0:64, cs], kT[:, cs], qT[:, cs], True, True, [kT.b(), qT.b()], [pA.b()])
        for c in range(NCH):
            cs = slice(c * 64, (c + 1) * 64)
            k.stt(WT[0:64, cs], pA[0:64, cs], g[:, 64 + c * 4 + h:65 + c * 4 + h], C["m_incl"][0:64, 0:64], ALU.mult, ALU.mult,
                  [pA.b(), g.b(), C["m_incl"].b()], [WT.b()])
        pKV = [k.ps(), k.ps()]
        for c in range(NCH):
            pk = pKV[c // 2]
            o = (c % 2) * 132
            k.mm(pk[:, o:o + 129], Kg[0:64, c * 128:(c + 1) * 128], Vx3[:, c, :], True, True, [Kg.b(), Vx.b()], [pk.b()])
        NUM3 = v3(NUM[0:64, 0:NCH * 129], NCH)
        for c in range(NCH):
            cs = slice(c * 64, (c + 1) * 64)
            pn = k.ps()
            k.mm(pn[0:64, 0:129], qT[:, cs], Ct[:, 0:129], True, False, [qT.b(), Ct.b()], [pn.b()])
            k.mm(pn[0:64, 0:129], WT[0:64, cs], Vx3[:, c, :], False, True, [WT.b(), Vx.b()], [pn.b()])
            k.cp("act", NUM3[:, c, :], pn[0:64, 0:129], [pn.b()], [NUM.b()])
            ef = efl[:, c * 4 + h:c * 4 + h + 1]
            k.ts("dve", Ct[:, 0:129], Ct[:, 0:129], ef, None, ALU.mult, None, [Ct.b(), efl.b()], [Ct.b()])
            pk = pKV[c // 2]
            o = (c % 2) * 132
            k.stt(Ct[:, 0:129], pk[:, o:o + 129], ef, Ct[:, 0:129], ALU.mult, ALU.add, [pk.b(), efl.b(), Ct.b()], [Ct.b()])
        EFh = v3(g[:, 80:96], NCH)[:, :, h]
        d = sm[0:64, 32:32 + NCH]
        k.tt("dve", d, NUM3[:, :, 128], EFh, ALU.mult, [NUM.b(), g.b()], [sm.b()])
        d2 = sm[0:64, 52:52 + NCH]
        k.ts("dve", d2, d, -1.0, None, ALU.mult, None, [sm.b()], [sm.b()])
        k.tt("dve", d, d, d2, ALU.max, [sm.b()], [sm.b()])
        k.ts("dve", d, d, 1.0, None, ALU.max, None, [sm.b()], [sm.b()])
        fw.op("dve", lambda e, d=d: e.reciprocal(out=d, in_=d), [sm.b()], [sm.b()])
        k.tt("dve", d, d, EFh, ALU.mult, [sm.b(), g.b()], [sm.b()])
        H3 = NUM3[:, :, 0:128]
        bsc = lambda a: v3(a, NCH).broadcast_to([64, NCH, 128])
        k.tt("dve", H3, H3, bsc(d), ALU.mult, [NUM.b(), sm.b()], [NUM.b()])
        s1, s2, mean, rstd = sm[0:64, 36:40], sm[0:64, 40:44], sm[0:64, 44:48], sm[0:64, 48:52]
        fw.op("dve", lambda e, H3=H3, s1=s1: e.tensor_reduce(out=s1, in_=H3, axis=AX.X, op=ALU.add), [NUM.b()], [sm.b()])
        sq3 = v3(sq[0:64, 0:NCH * 128], NCH)
        k.tt("dve", sq3, H3, H3, ALU.mult, [NUM.b()], [sq.b()])
        fw.op("dve", lambda e, sq3=sq3, s2=s2: e.tensor_reduce(out=s2, in_=sq3, axis=AX.X, op=ALU.add), [sq.b()], [sm.b()])
        k.ts("dve", mean, s1, 1.0 / 128, None, ALU.mult, None, [sm.b()], [sm.b()])
        k.tt("dve", s1, mean, mean, ALU.mult, [sm.b()], [sm.b()])
        k.ts("dve", s2, s2, 1.0 / 128, NORM_EPS, ALU.mult, ALU.add, [sm.b()], [sm.b()])
        k.tt("dve", s2, s2, s1, ALU.subtract, [sm.b()], [sm.b()])
        k.act(s2, s2, AF.Sqrt, [sm.b()], [sm.b()])
        fw.op("dve", lambda e, s2=s2, rstd=rstd: e.reciprocal(out=rstd, in_=s2), [sm.b()], [sm.b()])
        k.tt("dve", H3, H3, bsc(mean), ALU.subtract, [NUM.b(), sm.b()], [NUM.b()])
        k.tt("dve", sq3, H3, bsc(rstd), ALU.mult, [NUM.b(), sm.b()], [sq.b()])
        pT = k.ps()
        for c in range(NCH):
            k.tr(pT[:, c * 64:(c + 1) * 64], sq[0:64, c * 128:(c + 1) * 128], ident[0:64, 0:64], [sq.b(), ident.b()], [pT.b()])
        k.stt(YT[:, h, :], pT[:, 0:TBA], cp_(k, "ml_nw", h), so[:, 0:TBA], ALU.mult, ALU.mult, [pT.b(), A["colp"].b(), so.b()], [YT.b()])


def rwkv_setup(k, layer, st):
    fw = k.fw
    A = k.A
    dr = k.dram
    A["rw_ST"] = [k.sb("rwST%d_%d" % (layer, j), [128, 128], F32, st) for j in range(4)]
    for j in range(4):
        fw.op("pool", lambda e, j=j: e.memset(A["rw_ST"][j][:], 0.0), [], [A["rw_ST"][j].b()])
    A["rw_halo"] = k.sb("rwhalo%d" % layer, [128, 16], F32, st)
    fw.op("pool", lambda e: e.memset(A["rw_halo"][:], 0.0), [], [A["rw_halo"].b()])
    wa = k.sb("rwwa%d" % layer, [128, W], F32, st)
    A["rw_wa"] = wa
    fw.dma("sp", wa[0:64, :], dr["rw_w2"][layer * 64:(layer + 1) * 64, :], writes=[wa.b()])
    fw.dma("sp", wa[64:128, :], dr["rw_a2"][layer * 64:(layer + 1) * 64, :], writes=[wa.b()])
    g2 = k.sb("rwg2%d" % layer, [128, W], F32, st)
    A["rw_g2"] = g2
    fw.dma("sp", g2[:], dr["rw_g2"][layer * 128:(layer + 1) * 128, :], writes=[g2.b()])
    if layer > 0:
        v2 = k.sb("rwv2%d" % layer, [32, W], F32, st)
        A["rw_v2"] = v2
        fw.dma("sp", v2[:], dr["rw_v2"][(layer - 1) * 32:layer * 32, :], writes=[v2.b()])
    c = k.sb("rwc%d" % layer, [128, 8], F32, st)
    A["rw_c"] = c
    o, _ = COLP["k_a"]
    k.ts("dve", c[:, 0:4], A["colp"][:, o:o + 4], -1.0, 1.0, ALU.mult, ALU.add, [A["colp"].b()], [c.b()])


def rwkv_block(k, layer, tb):
    fw = k.fw
    A = k.A
    S = A["S"]
    C = k.C
    sm = A["sm"]
    dr = k.dram
    YT = A["YT"][0]
    NCH = TBA // 64
    ident = C["ident"]
    halo = A["rw_halo"]
    colp = A["colp"]
    Lo = lambda t: t[:, 0:TBA]
    Hi = lambda t: t[:, 256:256 + TBA]
    T0 = tb * TBA

    def lerp(p, dst, dT, zt, ci, nparts=128):
        P_ = slice(0, nparts)
        k.cp("dve", zt[P_, 0:1], halo[P_, ci:ci + 1], [halo.b()], [zt.b()])
        k.cp("act", zt[P_, 1:1 + TBA], p[P_, 0:TBA], [p.b()], [zt.b()])
        k.cp("dve", halo[P_, ci:ci + 1], zt[P_, TBA:TBA + 1], [zt.b()], [halo.b()])
        k.tt("dve", dst, zt[P_, 0:TBA], zt[P_, 1:1 + TBA], ALU.subtract, [zt.b()], [dT.b()])
        k.stt(dst, dst, cp_(k, "mu", ci), zt[P_, 1:1 + TBA], ALU.mult, ALU.add, [dT.b(), colp.b(), zt.b()], [dT.b()])

    pieces = [w_in_ap(k, layer, 1536, 256)]
    if layer > 0:
        pieces.append(w_in_ap(k, layer, N_COLS_BASE, 32))
    wsv, wsb = load_w_multi(k, pieces, 8)
    ztmp = S[5]
    Z12, SG = S[0], S[0]
    p = zt_mm(k, wsv, wsb, 0, 128, tb)
    lerp(p, Lo(Z12), Z12, ztmp, 12)
    k.act(Z12[0:64, 0:TBA], Z12[0:64, 0:TBA], AF.Tanh, [Z12.b()], [Z12.b()])
    p = zt_mm(k, wsv, wsb, 128, 128, tb)
    lerp(p, Hi(SG), SG, ztmp, 13)
    k.act(Hi(SG), Hi(SG), AF.Sigmoid, [SG.b()], [SG.b()])
    ZX = S[1]
    if layer > 0:
        p = zt_mm(k, wsv, wsb, 256, 32, tb)
        k.cp("act", ZX[0:32, 0:TBA], p[0:32, 0:TBA], [p.b()], [ZX.b()])
    wa, g2 = A["rw_wa"], A["rw_g2"]
    for j in range(4):
        js = slice(j * 128, (j + 1) * 128)
        wv, wb_ = load_w_multi(k, [w_in_ap(k, layer, q * W + j * 128, 128) for q in range(3)], 8)
        zt, R, Kp, V, LP, AS, KK, E1, E2, E3, TMP = S[5], S[6], S[7], S[8], S[9], S[10], S[11], S[12], S[13], S[14], S[15]
        AB, KR, BG = S[2], S[3], S[4]
        AT_, BT_, KT_, RT_, BON, GG = Lo(AB), Hi(AB), Lo(KR), Hi(KR), Lo(BG), Hi(BG)
        KM = Hi(S[1])
        p = zt_mm(k, wv, wb_, 0, 128, tb)
        lerp(p, Lo(R), R, zt, j)
        p = zt_mm(k, wv, wb_, 128, 128, tb)
        lerp(p, Lo(Kp), Kp, zt, 4 + j)
        p = zt_mm(k, wv, wb_, 256, 128, tb)
        lerp(p, Lo(V), V, zt, 8 + j)
        pw = k.ps()
        k.mm(pw[:, 0:TBA], wa[0:64, js], Z12[0:64, 0:TBA], True, True, [wa.b(), Z12.b()], [pw.b()])
        k.act(Lo(LP), pw[:, 0:TBA], AF.Sigmoid, [pw.b(), colp.b()], [LP.b()], bias=cp_(k, "w0", j))
        k.ts("dve", Lo(LP), Lo(LP), -0.6065306597126334, None, ALU.mult, None, [LP.b()], [LP.b()])
        pa = k.ps()
        k.mm(pa[:, 0:TBA], wa[64:128, js], Z12[64:128, 0:TBA], True, True, [wa.b(), Z12.b()], [pa.b()])
        k.act(Lo(AS), pa[:, 0:TBA], AF.Sigmoid, [pa.b(), colp.b()], [AS.b()], bias=cp_(k, "a0", j))
        pg = k.ps()
        k.mm(pg[:, 0:TBA], g2[:, js], Hi(SG), True, True, [g2.b(), SG.b()], [pg.b()])
        k.cp("act", GG, pg[:, 0:TBA], [pg.b()], [BG.b()])
        vf_ap = k.vf[j * 128:(j + 1) * 128, T0:T0 + TBA]
        if layer == 0:
            if not k.cfg.get("novf"):
                fw.dma("sp", vf_ap, Lo(V), reads=[V.b()], writes=[k.vfb])
        else:
            pv = k.ps()
            k.mm(pv[:, 0:TBA], A["rw_v2"][:, js], ZX[0:32, 0:TBA], True, True, [A["rw_v2"].b(), ZX.b()], [pv.b()])
            k.act(Lo(TMP), pv[:, 0:TBA], AF.Sigmoid, [pv.b(), colp.b()], [TMP.b()], bias=cp_(k, "v0", j))
            fw.dma("sp", Lo(E1), vf_ap, reads=[k.vfb], writes=[E1.b()])
            k.tt("dve", Lo(E1), Lo(E1), Lo(V), ALU.subtract, [E1.b(), V.b()], [E1.b()])
            k.tt("dve", Lo(E1), Lo(E1), Lo(TMP), ALU.mult, [E1.b(), TMP.b()], [E1.b()])
            k.tt("dve", Lo(V), Lo(V), Lo(E1), ALU.add, [V.b(), E1.b()], [V.b()])
        k.ts("dve", Lo(KK), Lo(Kp), cp_(k, "k_k", j), None, ALU.mult, None, [Kp.b(), colp.b()], [KK.b()])
        k.tt("dve", Lo(TMP), Lo(KK), Lo(KK), ALU.mult, [KK.b()], [TMP.b()])
        pn = k.ps()
        k.mm(pn[:, 0:TBA], C["bd64"][:], Lo(TMP), True, True, [C["bd64"].b(), TMP.b()], [pn.b()])
        k.act(Lo(TMP), pn[:, 0:TBA], AF.Sqrt, [pn.b()], [TMP.b()])
        k.ts("dve", Lo(TMP), Lo(TMP), 1e-12, None, ALU.max, None, [TMP.b()], [TMP.b()])
        fw.op("dve", lambda e, TMP=TMP: e.reciprocal(out=Lo(TMP), in_=Lo(TMP)), [TMP.b()], [TMP.b()])
        k.tt("dve", Lo(KK), Lo(KK), Lo(TMP), ALU.mult, [KK.b(), TMP.b()], [KK.b()])
        k.ts("dve", KM, Lo(AS), cp_(k, "k_a", j), A["rw_c"][:, j:j + 1], ALU.mult, ALU.add, [AS.b(), colp.b(), A["rw_c"].b()], [S[1].b()])
        k.tt("dve", KM, KM, Lo(Kp), ALU.mult, [S[1].b(), Kp.b()], [S[1].b()])
        k.stt(Lo(TMP), Lo(R), cp_(k, "r_k", j), KM, ALU.mult, ALU.mult, [R.b(), colp.b(), S[1].b()], [TMP.b()])
        pb = k.ps()
        k.mm(pb[:, 0:TBA], C["bd64"][:], Lo(TMP), True, True, [C["bd64"].b(), TMP.b()], [pb.b()])
        k.tt("dve", BON, pb[:, 0:TBA], Lo(V), ALU.mult, [pb.b(), V.b()], [BG.b()])
        k.cp("dve", Lo(E3), Lo(LP), [LP.b()], [E3.b()])
        fw.op("dve", lambda e, LP=LP: e.tensor_tensor_scan(out=Lo(LP), data0=C["rmask"][:, 0:TBA], data1=Lo(LP), initial=0.0,
                                                            op0=ALU.mult, op1=ALU.add), [LP.b(), C["rmask"].b()], [LP.b()])
        k.tt("dve", Lo(E3), Lo(LP), Lo(E3), ALU.subtract, [LP.b(), E3.b()], [E3.b()])
        k.act(Lo(E3), Lo(E3), AF.Exp, [E3.b()], [E3.b()])
        k.act(Lo(E1), Lo(LP), AF.Exp, [LP.b()], [E1.b()])
        k.act(Lo(E2), Lo(LP), AF.Exp, [LP.b()], [E2.b()], scale=-1.0)
        pc = sm[:, 56:56 + NCH]
        k.cp("dve", pc, v3(Lo(E1), NCH)[:, :, 63], [E1.b()], [sm.b()])
        k.stt(AT_, Lo(KK), -1.0, Lo(E3), ALU.mult, ALU.mult, [KK.b(), E3.b()], [AB.b()])
        k.tt("dve", BT_, Lo(KK), Lo(AS), ALU.mult, [KK.b(), AS.b()], [AB.b()])
        k.tt("dve", BT_, BT_, Lo(E2), ALU.mult, [AB.b(), E2.b()], [AB.b()])
        k.tt("dve", KT_, KM, Lo(E2), ALU.mult, [S[1].b(), E2.b()], [KR.b()])
        k.tt("dve", RT_, Lo(R), Lo(E1), ALU.mult, [R.b(), E1.b()], [KR.b()])
        if k.cfg.get("rwstop") == "prep":
            continue
        Bt, Kt, Vt = S[5], S[6], S[7]
        for src_ap, srcb, dst in ((BT_, AB, Bt), (KT_, KR, Kt), (Lo(V), V, Vt)):
            p = k.ps()
            for c in range(NCH):
                k.tr(p[0:64, c * 128:(c + 1) * 128], src_ap[:, c * 64:(c + 1) * 64], ident[:], [srcb.b(), ident.b()], [p.b()])
            k.cp("act", dst[0:64, 0:NCH * 128], p[0:64, 0:NCH * 128], [p.b()], [dst.b()])
        if k.cfg.get("rwstop") == "tok":
            continue
        AK, RB, RK, Nm, NTm, Xm = S[8], S[9], S[10], S[11], S[12], S[13]
        NI = NCH * 2

        Mt = [S[13], S[14], S[15]]
        for mi, (src_ap, srcb) in enumerate(((AT_, AB), (BT_, AB), (RT_, KR))):
            for hh in range(2):
                dst_ap = Lo(Mt[mi]) if hh == 0 else Hi(Mt[mi])
                k.ts("dve", dst_ap, src_ap, C["bd64"][:, hh * 64:hh * 64 + 1], None, ALU.mult, None, [srcb.b(), C["bd64"].b()], [Mt[mi].b()])
        ATm = (Lo(Mt[0]), Hi(Mt[0]))
        BTm = (Lo(Mt[1]), Hi(Mt[1]))
        RTm = (Lo(Mt[2]), Hi(Mt[2]))

        def amat(lhs_ap, lb, rhs_m, rb, dst, mask):
            p = k.ps()
            for c in range(NCH):
                for hh in range(2):
                    i = c * 2 + hh
                    cs = slice(c * 64, (c + 1) * 64)
                    k.mm(p[0:64, i * 64:(i + 1) * 64], lhs_ap[:, cs], rhs_m[hh][:, cs], True, True, [lb.b(), rb.b()], [p.b()])
            k.tt("dve", dst[0:64, 0:NI * 64], p[0:64, 0:NI * 64], C[mask][0:64, 0:NI * 64], ALU.mult, [p.b(), C[mask].b()], [dst.b()])
        amat(BT_, AB, ATm, Mt[0], Nm, "m_strict")
        amat(AT_, AB, BTm, Mt[1], NTm, "m_lows")
        amat(KT_, KR, ATm, Mt[0], AK, "m_strict")
        amat(BT_, AB, RTm, Mt[2], RB, "m_incl")
        amat(KT_, KR, RTm, Mt[2], RK, "m_incl")
        if k.cfg.get("rwstop") == "amat":
            continue
        idb = ident[0:64, 0:64].unsqueeze(1).broadcast_to([64, NI, 64])
        k.tt("dve", v3(Xm[0:64, 0:NI * 64], NI), v3(Nm[0:64, 0:NI * 64], NI), idb, ALU.add, [Nm.b(), ident.b()], [Xm.b()])
        for lvl in range(5):
            pP = k.ps() if lvl < 4 else None
            pPT = k.ps()
            for i in range(NI):
                cs = slice(i * 64, (i + 1) * 64)
                if pP is not None:
                    k.mm(pP[0:64, cs], NTm[0:64, cs], Nm[0:64, cs], True, True, [NTm.b(), Nm.b()], [pP.b()])
                k.mm(pPT[0:64, cs], Nm[0:64, cs], NTm[0:64, cs], True, True, [NTm.b(), Nm.b()], [pPT.b()])
            if pP is not None:
                k.cp("act", Nm[0:64, 0:NI * 64], pP[0:64, 0:NI * 64], [pP.b()], [Nm.b()])
            k.cp("dve", NTm[0:64, 0:NI * 64], pPT[0:64, 0:NI * 64], [pPT.b()], [NTm.b()])
            pX = k.ps()
            for i in range(NI):
                cs = slice(i * 64, (i + 1) * 64)
                k.mm(pX[0:64, cs], NTm[0:64, cs], Xm[0:64, cs], True, True, [NTm.b(), Xm.b()], [pX.b()])
            k.tt("dve", Xm[0:64, 0:NI * 64], Xm[0:64, 0:NI * 64], pX[0:64, 0:NI * 64], ALU.add, [Xm.b(), pX.b()], [Xm.b()])
        if k.cfg.get("rwstop") == "inv":
            continue
        ST = A["rw_ST"][j]
        Yb, Ws = S[14], S[15]
        for c in range(NCH):
            cs = slice(c * 64, (c + 1) * 64)
            tok = slice(c * 128, (c + 1) * 128)
            pw0 = k.ps()
            k.mm(pw0[0:64, 0:128], AT_[:, cs], ST[:], True, False, [AB.b(), ST.b()], [pw0.b()])
            for hh in range(2):
                i = c * 2 + hh
                hv = slice(hh * 64, (hh + 1) * 64)
                k.mm(pw0[0:64, hv], AK[0:64, i * 64:(i + 1) * 64], Vt[0:64, c * 128 + hh * 64:c * 128 + (hh + 1) * 64], False, hh == 1,
                     [AK.b(), Vt.b()], [pw0.b()])
            k.cp("act", Ws[0:64, 0:128], pw0[0:64, 0:128], [pw0.b()], [Ws.b()])
            pu = k.ps()
            for hh in range(2):
                i = c * 2 + hh
                hv = slice(hh * 64, (hh + 1) * 64)
                k.mm(pu[0:64, hv], Xm[0:64, i * 64:(i + 1) * 64], Ws[0:64, hv], True, True, [Xm.b(), Ws.b()], [pu.b()])
            k.cp("act", Ws[0:64, 128:256], pu[0:64, 0:128], [pu.b()], [Ws.b()])
            Us = Ws[0:64, 128:256]
            py = k.ps()
            k.mm(py[0:64, 0:128], RT_[:, cs], ST[:], True, False, [KR.b(), ST.b()], [py.b()])
            for hh in range(2):
                i = c * 2 + hh
                hv = slice(hh * 64, (hh + 1) * 64)
                k.mm(py[0:64, hv], RB[0:64, i * 64:(i + 1) * 64], Ws[0:64, 128 + hh * 64:128 + (hh + 1) * 64], False, False,
                     [RB.b(), Ws.b()], [py.b()])
                k.mm(py[0:64, hv], RK[0:64, i * 64:(i + 1) * 64], Vt[0:64, c * 128 + hh * 64:c * 128 + (hh + 1) * 64], False, hh == 1,
                     [RK.b(), Vt.b()], [py.b()])
            k.cp("act", Yb[0:64, tok], py[0:64, 0:128], [py.b()], [Yb.b()])
            pS = k.ps()
            k.mm(pS[:, 0:128], Bt[0:64, tok], Us, True, False, [Bt.b(), Ws.b()], [pS.b()])
            k.mm(pS[:, 0:128], Kt[0:64, tok], Vt[0:64, tok], False, True, [Kt.b(), Vt.b()], [pS.b()])
            for hh in range(2):
                P_ = slice(hh * 64, (hh + 1) * 64)
                k.ts("dve", ST[P_, P_], ST[P_, P_], sm[P_, 56 + c:57 + c], None, ALU.mult, None, [ST.b(), sm.b()], [ST.b()])
                k.stt(ST[P_, P_], pS[P_, P_], sm[P_, 56 + c:57 + c], ST[P_, P_], ALU.mult, ALU.add, [pS.b(), sm.b(), ST.b()], [ST.b()])
        if k.cfg.get("rwstop") == "seq":
            continue
        NG = NCH * 2
        Y3 = v3(Yb[0:64, 0:NG * 64], NG)
        sqt = S[8]
        sq3 = v3(sqt[0:64, 0:NG * 64], NG)
        s1, s2, mean, rstd = sm[0:64, 0:8], sm[0:64, 8:16], sm[0:64, 16:24], sm[0:64, 24:32]
        bsc = lambda a: v3(a, NG).broadcast_to([64, NG, 64])
        fw.op("dve", lambda e, Y3=Y3, s1=s1: e.tensor_reduce(out=s1, in_=Y3, axis=AX.X, op=ALU.add), [Yb.b()], [sm.b()])
        k.tt("dve", sq3, Y3, Y3, ALU.mult, [Yb.b()], [sqt.b()])
        fw.op("dve", lambda e, sq3=sq3, s2=s2: e.tensor_reduce(out=s2, in_=sq3, axis=AX.X, op=ALU.add), [sqt.b()], [sm.b()])
        k.ts("dve", mean, s1, 1.0 / 64, None, ALU.mult, None, [sm.b()], [sm.b()])
        k.tt("dve", s1, mean, mean, ALU.mult, [sm.b()], [sm.b()])
        k.ts("dve", s2, s2, 1.0 / 64, 64e-5, ALU.mult, ALU.add, [sm.b()], [sm.b()])
        k.tt("dve", s2, s2, s1, ALU.subtract, [sm.b()], [sm.b()])
        k.act(s2, s2, AF.Sqrt, [sm.b()], [sm.b()])
        fw.op("dve", lambda e, s2=s2, rstd=rstd: e.reciprocal(out=rstd, in_=s2), [sm.b()], [sm.b()])
        k.tt("dve", Y3, Y3, bsc(mean), ALU.subtract, [Yb.b(), sm.b()], [Yb.b()])
        k.tt("dve", Y3, Y3, bsc(rstd), ALU.mult, [Yb.b(), sm.b()], [Yb.b()])
        pT = k.ps()
        for c in range(NCH):
            k.tr(pT[:, c * 64:(c + 1) * 64], Yb[0:64, c * 128:(c + 1) * 128], ident[0:64, 0:64], [Yb.b(), ident.b()], [pT.b()])
        k.ts("dve", Lo(TMP), pT[:, 0:TBA], cp_(k, "lnx_g", j), cp_(k, "lnx_b", j), ALU.mult, ALU.add, [pT.b(), colp.b()], [TMP.b()])
        k.tt("dve", Lo(TMP), Lo(TMP), BON, ALU.add, [TMP.b(), BG.b()], [TMP.b()])
        k.tt("dve", YT[:, j, :], Lo(TMP), GG, ALU.mult, [TMP.b(), BG.b()], [YT.b()])
```

```python
import contextlib
import numpy as np
import concourse.bass as bass
import concourse.mybir as mybir
from concourse.bass_utils import run_bass_kernel_spmd

F32 = mybir.dt.float32
BF16 = mybir.dt.bfloat16
AF = mybir.ActivationFunctionType
ALU = mybir.AluOpType
AX = mybir.AxisListType

D = 1024
SEQ = 2048
DEPTH = 4
NT = SEQ // 128
W = 512
NEXP = 32
DPLE = 256
ALPHA = float((2 * DEPTH) ** 0.25)
LN_EPS = 1e-5
NORM_EPS = 1e-6
RWKV_COLS = 1792
N_COLS_BASE = 11016
N_COLS_REST = 11048
TB = 512
NB = SEQ // TB
TBA = 256
NBA = SEQ // TBA

ENG_NAMES = ["pe", "act", "dve", "pool", "sp"]


class Buf:
    __slots__ = ("name", "w", "r", "excl")

    def __init__(self, name="", excl=False):
        self.name = name
        self.w = {}
        self.r = {}
        self.excl = excl


class FW:
    def __init__(self, nc, sems, n_dma_sems):
        self.nc = nc
        self.sems = sems
        self.ops = {e: [] for e in ENG_NAMES}
        self.cnt = {e: 0 for e in ENG_NAMES}
        self.waited = {e: {} for e in ENG_NAMES}
        self.n_dma_sems = n_dma_sems
        self.dma_cnt = [0] * n_dma_sems
        half = n_dma_sems // 2
        self.dma_rng = {"sp": (0, half), "act": (0, half), "pool": (half, n_dma_sems)}
        self.dma_rr = {"sp": 0, "act": 0, "pool": half}
        self.n_instr = 0
        self.final_tokens = []

    def _need(self, eng, deps):
        for key, val in deps.items():
            if key == ("e", "pe") and eng == "pe":
                continue
            if self.waited[eng].get(key, 0) >= val:
                continue
            self.waited[eng][key] = val
            self.ops[eng].append(("wait", key, val))

    @staticmethod
    def _collect(reads, writes):
        deps = {}
        for b in reads:
            for k, v in b.w.items():
                if deps.get(k, 0) < v:
                    deps[k] = v
        for b in writes:
            for k, v in b.w.items():
                if deps.get(k, 0) < v:
                    deps[k] = v
            for k, v in b.r.items():
                if deps.get(k, 0) < v:
                    deps[k] = v
        return deps

    @staticmethod
    def _mark(tok, reads, writes):
        k, v = tok
        for b in reads:
            if b.r.get(k, 0) < v:
                b.r[k] = v
        for b in writes:
            if b.w.get(k, 0) < v:
                b.w[k] = v

    def op(self, eng, fn, reads=(), writes=()):
        ex = [b for b in reads if b.excl]
        if ex:
            reads = [b for b in reads if not b.excl]
            writes = list(writes) + ex
        self._need(eng, self._collect(reads, writes))
        self.cnt[eng] += 1
        tok = (("e", eng), self.cnt[eng])
        self.ops[eng].append(("op", fn))
        self._mark(tok, reads, writes)
        self.n_instr += 1
        return tok

    def dma(self, q, out, in_, reads=(), writes=(), final=False):
        deps = self._collect(reads, writes)
        lo, hi = self.dma_rng[q]
        rk = "pool" if q == "pool" else "sp"
        idx = self.dma_rr[rk]
        self.dma_rr[rk] = lo + (idx + 1 - lo) % (hi - lo)
        key = ("d", idx)
        if self.dma_cnt[idx] > 0:
            prev = self.dma_cnt[idx] * 16
            if deps.get(key, 0) < prev:
                deps[key] = prev
        self._need(q, deps)
        self.dma_cnt[idx] += 1
        tok = (key, self.dma_cnt[idx] * 16)
        self.ops[q].append(("dma", out, in_, key))
        self._mark(tok, reads, writes)
        if final:
            self.final_tokens.append(tok)
        self.n_instr += 1
        return tok

    def flush(self, final=False):
        allv = {("e", e): self.cnt[e] for e in ENG_NAMES if self.cnt[e] > 0}
        for i in range(self.n_dma_sems):
            if self.dma_cnt[i] > 0:
                allv[("d", i)] = self.dma_cnt[i] * 16
        for e in ENG_NAMES:
            d = dict(allv)
            d.pop(("e", e), None)
            self._need(e, d)
        nc = self.nc
        sems = self.sems
        with nc.Block() as block:
            regs = {"pe": block.tensor, "act": block.scalar, "dve": block.vector,
                    "pool": block.gpsimd, "sp": block.sync}

            def make(e):
                items = self.ops[e]

                def body(eng):
                    mysem = sems[("e", e)]
                    for item in items:
                        kind = item[0]
                        if kind == "wait":
                            eng.wait_ge(sems[item[1]], item[2])
                        elif kind == "op":
                            item[1](eng).then_inc(mysem, 1)
                        else:
                            eng.dma_start(out=item[1], in_=item[2]).then_inc(sems[item[3]], 16)
                return body
            for e in ENG_NAMES:
                if self.ops[e]:
                    regs[e](make(e))
        self.ops = {e: [] for e in ENG_NAMES}


class T:
    def __init__(self, t, excl=False):
        self.t = t
        self.bufs = {}
        self.excl = excl

    def b(self, key=0):
        if key not in self.bufs:
            self.bufs[key] = Buf(excl=self.excl)
        return self.bufs[key]

    def __getitem__(self, idx):
        return self.t[idx]


class K:
    def __init__(self, cfg):
        self.cfg = cfg
        self.nc = bass.Bass("TRN2", target_bir_lowering=False)
        self.st = contextlib.ExitStack()
        self.dram = {}
        self.NDS = 32

    def din(self, name, shape, dtype=F32):
        self.dram[name] = self.nc.dram_tensor(name, list(shape), dtype, kind="ExternalInput").ap()
        return self.dram[name]

    def dout(self, name, shape, dtype=F32):
        self.dram[name] = self.nc.dram_tensor(name, list(shape), dtype, kind="ExternalOutput").ap()
        return self.dram[name]

    def sb(self, name, shape, dtype=F32, stack=None):
        t = (stack or self.st).enter_context(self.nc.sbuf_tensor(name, list(shape), dtype))
        return T(t)

    def setup(self):
        nc = self.nc
        st = self.st
        sems = {}
        for e in ENG_NAMES:
            sems[("e", e)] = st.enter_context(nc.semaphore("s_" + e))
        for i in range(self.NDS):
            sems[("d", i)] = st.enter_context(nc.semaphore("sd%d" % i))
        self.fw = FW(nc, sems, self.NDS)
        self.psum = []
        for i in range(8):
            p = st.enter_context(nc.psum_tensor("ps%d" % i, [128, 512], F32))
            self.psum.append(T(p, excl=True))
        self.ps_rr = 0

    def ps(self):
        p = self.psum[self.ps_rr]
        self.ps_rr = (self.ps_rr + 1) % 8
        return p

    def mm(self, out, lhsT, rhs, start, stop, reads, writes):
        self.fw.op("pe", lambda e: e.matmul(out, lhsT=lhsT, rhs=rhs, start=start, stop=stop), reads, writes)

    def tr(self, out, in_, ident, reads, writes):
        self.fw.op("pe", lambda e: e.transpose(out, in_, ident), reads, writes)

    def act(self, out, in_, func, reads, writes, bias=None, scale=None, eng="act", accum_out=None):
        kw = {}
        if bias is not None:
            kw["bias"] = bias
        if scale is not None:
            kw["scale"] = scale
        if accum_out is not None:
            kw["accum_out"] = accum_out
        self.fw.op("act", lambda e: e.activation(out=out, in_=in_, func=func, **kw), reads, writes)

    def ts(self, eng, out, in0, s1, s2, op0, op1, reads, writes):
        if op1 is None:
            self.fw.op(eng, lambda e: e.tensor_scalar(out=out, in0=in0, scalar1=s1, scalar2=None, op0=op0), reads, writes)
        else:
            self.fw.op(eng, lambda e: e.tensor_scalar(out=out, in0=in0, scalar1=s1, scalar2=s2, op0=op0, op1=op1), reads, writes)

    def tt(self, eng, out, in0, in1, op, reads, writes):
        self.fw.op(eng, lambda e: e.tensor_tensor(out=out, in0=in0, in1=in1, op=op), reads, writes)

    def stt(self, out, in0, scalar, in1, op0, op1, reads, writes):
        self.fw.op("dve", lambda e: e.scalar_tensor_tensor(out=out, in0=in0, scalar=scalar, in1=in1, op0=op0, op1=op1), reads, writes)

    def cp(self, eng, out, in_, reads, writes):
        if eng == "act":
            self.fw.op("act", lambda e: e.activation(out=out, in_=in_, func=AF.Copy), reads, writes)
        else:
            self.fw.op(eng, lambda e: e.tensor_copy(out=out, in_=in_), reads, writes)


def _cols(v):
    v = np.asarray(v, np.float32)
    return np.ascontiguousarray(v.reshape(-1, 128).T)


def host_consts():
    c = {}
    c["ident"] = np.eye(128, dtype=np.float32)
    s = np.arange(64)
    c["m_incl"] = np.tile((s[:, None] <= s[None, :]).astype(np.float32), (1, 8))
    c["m_strict"] = np.tile((s[:, None] < s[None, :]).astype(np.float32), (1, 8))
    c["m_lows"] = np.tile((s[:, None] > s[None, :]).astype(np.float32), (1, 8))
    c["ones"] = np.ones((128, 128), np.float32)
    bd = np.zeros((128, 128), np.float32)
    bd[:64, :64] = 1.0
    bd[64:, 64:] = 1.0
    c["bd64"] = bd
    rm = np.ones((128, 512), np.float32)
    rm[:, ::64] = 0.0
    c["rmask"] = rm
    return c


def build_inmaps(inputs):
    f = lambda a: np.ascontiguousarray(np.asarray(a, np.float32))
    L = DEPTH
    shared = {}
    shared["w_in_first"] = f(inputs["w_in_first"])
    shared["w_in_rest"] = f(inputs["w_in_rest"]).reshape(3 * D, N_COLS_REST)
    shared["w_gu"] = f(inputs["expert_w_gu"]).reshape(-1, 2 * D)
    shared["w_dn"] = f(inputs["expert_w_down"]).reshape(-1, D)
    shared["b_dn"] = f(inputs["expert_b_down"]).reshape(L * NEXP, D)
    bgu = f(inputs["expert_b_gu"]).reshape(L, NEXP, 16, 128)
    shared["b_gu"] = np.ascontiguousarray(bgu.transpose(0, 3, 1, 2)).reshape(L * 128, NEXP * 16)
    shared["w_pg"] = f(inputs["ple_gate_w"]).reshape(L * D, D)
    shared["w_pp"] = f(inputs["ple_proj_w"]).reshape(L * DPLE, D)
    shared["w_out"] = f(inputs["w_out"]).reshape(L * D, D)
    shared["w_br"] = f(inputs["w_branch"]).reshape(L * 4 * W, D)
    shared["w_rt"] = f(inputs["router_w"]).reshape(L * D, NEXP)
    shared["b_rt"] = f(inputs["router_b"])
    shared["ln1_g"] = f(inputs["ln1_g"]); shared["ln1_b"] = f(inputs["ln1_b"])
    shared["ln2_g"] = f(inputs["ln2_g"]); shared["ln2_b"] = f(inputs["ln2_b"])
    for k, v in host_consts().items():
        shared["c_" + k] = v
    build_inmaps_a(inputs, shared)
    maps = []
    for b in range(8):
        m = dict(shared)
        m["x"] = f(inputs["x"][b])
        m["pT"] = np.ascontiguousarray(f(inputs["p"][:, b]).transpose(0, 2, 1)).reshape(L * DPLE, SEQ)
        maps.append(m)
    return maps


def declare_dram(k):
    L = DEPTH
    LE = k.cfg.get("declLE", L * NEXP)
    k.din("x", [SEQ, D]); k.din("pT", [L * DPLE, SEQ])
    k.din("w_in_first", [D, N_COLS_BASE]); k.din("w_in_rest", [k.cfg.get("declR", 3) * D, N_COLS_REST])
    k.din("w_gu", [LE * D, 2 * D]); k.din("w_dn", [LE * D, D]); k.din("b_dn", [L * NEXP, D])
    k.din("b_gu", [L * 128, NEXP * 16])
    k.din("w_pg", [L * D, D]); k.din("w_pp", [L * DPLE, D]); k.din("w_out", [L * D, D]); k.din("w_br", [L * 4 * W, D])
    k.din("w_rt", [L * D, NEXP]); k.din("b_rt", [L, NEXP])
    for n in ("ln1_g", "ln1_b", "ln2_g", "ln2_b"):
        k.din(n, [L, D])
    for n, v in host_consts().items():
        k.din("c_" + n, list(v.shape))
    declare_dram_a(k)
    k.dout("y", [SEQ, D])


def bcast_rows(ap_row, nparts):
    return ap_row.partition_broadcast(nparts) if hasattr(ap_row, "partition_broadcast") else ap_row


def load_consts(k):
    fw = k.fw
    C = {}
    for n, v in host_consts().items():
        t = k.sb("sc_" + n, list(v.shape))
        fw.dma("sp", t[:], k.dram["c_" + n], writes=[t.b()])
        C[n] = t
    k.C = C


def make_xT(k, tile, layer, with_router):
    fw = k.fw
    X, XT, C = k.X, k.XT, k.C
    tok = slice(tile * 128, (tile + 1) * 128)
    xtf = k.B_xtf if with_router else None
    for hb in range(2):
        p = k.ps()
        for cc in range(4):
            c = hb * 4 + cc
            k.tr(p[:, cc * 128:(cc + 1) * 128], X[:, tile, c * 128:(c + 1) * 128], C["ident"][:],
                 [X.b(tile), C["ident"].b()], [p.b()])
        src = p[:, :].rearrange("p (c t) -> p c t", c=4)
        k.cp("act", XT[:, hb * 4:(hb + 1) * 4, tok], src, [p.b()], [XT.b(tile)])
        if with_router:
            k.cp("dve", xtf[:, hb * 4:(hb + 1) * 4, :], src, [p.b()], [xtf.b()])
    xtv = k.cfg.get("xtv", "")
    if with_router and "nomm" not in xtv:
        p = k.ps()
        for c in range(8):
            k.mm(p[:, 0:NEXP], xtf[:, c, :], k.B_wrt[:, c, :], c == 0, c == 7, [xtf.b(), k.B_wrt.b()], [p.b()])
        if "nott" not in xtv:
            k.tt("dve", k.B_lg[:, tile, :], p[:, 0:NEXP], k.B_brt[:], ALU.add, [p.b(), k.B_brt.b()], [k.B_lg.b(tile)])


def wslot(k):
    s = k.ring_rr
    k.ring_rr = (k.ring_rr + 1) % k.NRING
    return s


def load_w(k, dram_ap_2d, kc, ncols, q="pool"):
    s = wslot(k)
    full = k.ring[s]
    view = full[:, 0:kc * ncols].rearrange("p (a c) -> p a c", a=kc)
    src = dram_ap_2d.rearrange("(a p) c -> p a c", p=128)
    k.fw.dma(q, view, src, writes=[k.ringb[s]])
    return view, k.ringb[s]


def phase_b(k, layer, last):
    fw = k.fw
    nc = k.nc
    X, XT, C = k.X, k.XT, k.C
    st = contextlib.ExitStack()
    dr = k.dram
    with st:
        k.NRING = 4
        k.ring = []
        k.ringb = []
        for i in range(k.NRING):
            t = st.enter_context(nc.sbuf_tensor("ringB%d_%d" % (layer, i), [128, 4096], BF16))
            k.ring.append(t)
            k.ringb.append(Buf())
        k.ring_rr = 0
        HT = k.sb("HT%d" % layer, [128, 8, SEQ], BF16, st)
        G = k.sb("G%d" % layer, [128, NT, NEXP], F32, st)
        G2 = k.sb("G2_%d" % layer, [128, D], F32, st)
        B2 = k.sb("B2_%d" % layer, [128, D], F32, st)
        BD = k.sb("BD%d" % layer, [NEXP, D], F32, st)
        BGU = k.sb("BGU%d" % layer, [128, NEXP * 16], F32, st)
        BU1 = k.sb("BU1_%d" % layer, [128, NEXP * 8], F32, st)
        NTMP = 6
        tmp = [k.sb("tmpB%d_%d" % (layer, i), [128, 512], F32, st) for i in range(NTMP)]
        sm = k.sb("smB%d" % layer, [128, 64], F32, st)
        tstate = {"i": 0}

        def T_():
            t = tmp[tstate["i"]]
            tstate["i"] = (tstate["i"] + 1) % NTMP
            return t

        fw.dma("sp", G2[:], dr["ln2_g"][layer:layer + 1, :].partition_broadcast(128), writes=[G2.b()])
        fw.dma("sp", B2[:], dr["ln2_b"][layer:layer + 1, :].partition_broadcast(128), writes=[B2.b()])
        fw.dma("sp", BD[:], dr["b_dn"][layer * NEXP:(layer + 1) * NEXP, :], writes=[BD.b()])
        fw.dma("sp", BGU[:], dr["b_gu"][layer * 128:(layer + 1) * 128, :], writes=[BGU.b()])
        bgu3 = BGU[:, :].rearrange("p (e c) -> p e c", c=16)
        k.ts("dve", BU1[:, :].rearrange("p (e c) -> p e c", c=8), bgu3[:, :, 8:16], 1.0, None, ALU.add, None,
             [BGU.b()], [BU1.b()])

        LG = k.B_lg
        skip = k.cfg.get('skip', ())
        for tile in range(NT if 'router' not in skip else 0):
            m8 = sm[:, 0:8]
            fw.op("dve", lambda e, tile=tile: e.max(out=sm[:, 0:8], in_=LG[:, tile, :]), [LG.b(tile)], [sm.b()])
            mask = T_()
            k.ts("dve", mask[:, 0:NEXP], LG[:, tile, :], sm[:, 3:4], None, ALU.is_ge, None, [LG.b(tile), sm.b()], [mask.b()])
            k.ts("dve", sm[:, 8:9], sm[:, 0:1], -1.0, None, ALU.mult, None, [sm.b()], [sm.b()])
            ex = T_()
            k.act(ex[:, 0:NEXP], LG[:, tile, :], AF.Exp, [LG.b(tile), sm.b()], [ex.b()], bias=sm[:, 8:9])
            k.tt("dve", ex[:, 0:NEXP], ex[:, 0:NEXP], mask[:, 0:NEXP], ALU.mult, [ex.b(), mask.b()], [ex.b()])
            fw.op("dve", lambda e, ex=ex: e.reduce_sum(out=sm[:, 9:10], in_=ex[:, 0:NEXP], axis=AX.X), [ex.b()], [sm.b()])
            fw.op("dve", lambda e: e.reciprocal(out=sm[:, 10:11], in_=sm[:, 9:10]), [sm.b()], [sm.b()])
            k.ts("dve", G[:, tile, :], ex[:, 0:NEXP], sm[:, 10:11], None, ALU.mult, None, [ex.b(), sm.b()], [G.b(tile)])

        for half in range(2 if 'ple' not in skip else 0):
            cs = slice(half * 512, (half + 1) * 512)
            wpg, bpg = load_w(k, dr["w_pg"][layer * D:(layer + 1) * D, cs], 8, 512)
            wpp, bpp = load_w(k, dr["w_pp"][layer * DPLE:(layer + 1) * DPLE, cs], 2, 512)
            ptv, bpt = load_w(k, dr["pT"][layer * DPLE:(layer + 1) * DPLE, :], 2, SEQ)
            for tile in range(NT):
                tok = slice(tile * 128, (tile + 1) * 128)
                pg = k.ps()
                for c in range(8):
                    k.mm(pg[:, :], XT[:, c, tok], wpg[:, c, :], c == 0, c == 7, [XT.b(tile), bpg], [pg.b()])
                pp = k.ps()
                for c in range(2):
                    k.mm(pp[:, :], ptv[:, c, tok], wpp[:, c, :], c == 0, c == 1, [bpt, bpp], [pp.b()])
                sg = T_()
                k.act(sg[:], pg[:, :], AF.Sigmoid, [pg.b()], [sg.b()])
                k.tt("dve", sg[:], sg[:], pp[:, :], ALU.mult, [sg.b(), pp.b()], [sg.b()])
                k.stt(X[:, tile, cs], X[:, tile, cs], ALPHA, sg[:], ALU.mult, ALU.add, [X.b(tile), sg.b()], [X.b(tile)])

        nexp = k.cfg.get("nexp", NEXP)
        for e in range(nexp):
            r0 = (layer * NEXP + e) * D
            wgu = dr["w_gu"][r0:r0 + D, :]
            wdn = dr["w_dn"][r0:r0 + D, :]
            for hp in range(2):
                wg, bg = load_w(k, wgu[:, hp * 512:(hp + 1) * 512], 8, 512)
                wu, bu = load_w(k, wgu[:, D + hp * 512:D + (hp + 1) * 512], 8, 512)
                for tb in range(NB):
                    ts_ = slice(tb * TB, (tb + 1) * TB)
                    xb = [XT.b(tb * 4 + i) for i in range(4)]
                    for jj in range(4):
                        j = hp * 4 + jj
                        js = slice(jj * 128, (jj + 1) * 128)
                        pG = k.ps()
                        for c in range(8):
                            k.mm(pG[:, :], wg[:, c, js], XT[:, c, ts_], c == 0, c == 7, xb + [bg], [pG.b()])
                        pU = k.ps()
                        for c in range(8):
                            k.mm(pU[:, :], wu[:, c, js], XT[:, c, ts_], c == 0, c == 7, xb + [bu], [pU.b()])
                        g = T_()
                        k.ts("dve", g[:], pG[:, :], BGU[:, e * 16 + j:e * 16 + j + 1], 7.0, ALU.add, ALU.min, [pG.b(), BGU.b()], [g.b()])
                        s = T_()
                        k.act(s[:], g[:], AF.Sigmoid, [g.b()], [s.b()], scale=1.702)
                        u1 = T_()
                        k.ts("dve", u1[:], pU[:, :], BU1[:, e * 8 + j:e * 8 + j + 1], 8.0, ALU.add, ALU.min, [pU.b(), BU1.b()], [u1.b()])
                        k.tt("dve", g[:], g[:], s[:], ALU.mult, [g.b(), s.b()], [g.b()])
                        k.stt(HT[:, j, ts_], u1[:], -6.0, g[:], ALU.max, ALU.mult, [u1.b(), g.b()], [HT.b(tb)])
            wd = []
            for half in range(2):
                wd.append(load_w(k, wdn[:, half * 512:(half + 1) * 512], 8, 512))
            for tile in range(NT):
                tok = slice(tile * 128, (tile + 1) * 128)
                for half in range(2):
                    cs = slice(half * 512, (half + 1) * 512)
                    pY = k.ps()
                    for c in range(8):
                        k.mm(pY[:, :], HT[:, c, tok], wd[half][0][:, c, :], c == 0, c == 7, [HT.b(tile // 4), wd[half][1]], [pY.b()])
                    k.stt(X[:, tile, cs], pY[:, :], G[:, tile, e:e + 1], X[:, tile, cs], ALU.mult, ALU.add,
                          [pY.b(), G.b(tile), X.b(tile)], [X.b(tile)])

        gt = k.sb("gtB%d" % layer, [NEXP, 128], F32, st)
        stt_ = k.sb("stB%d" % layer, [128, 16], F32, st)
        for tile in range(NT):
            if 'tail' in skip:
                fw.dma('sp', dr['y'][tile * 128:(tile + 1) * 128, :], X[:, tile, :], reads=[X.b(tile)], final=True)
                continue
            p = k.ps()
            k.tr(p[0:NEXP, 0:128], G[:, tile, :], C["ident"][:], [G.b(tile), C["ident"].b()], [p.b()])
            k.cp("act", gt[:], p[0:NEXP, 0:128], [p.b()], [gt.b()])
            for half in range(2):
                cs = slice(half * 512, (half + 1) * 512)
                pb = k.ps()
                k.mm(pb[:, :], gt[:], BD[:, cs], True, True, [gt.b(), BD.b()], [pb.b()])
                k.tt("dve", X[:, tile, cs], X[:, tile, cs], pb[:, :], ALU.add, [X.b(tile), pb.b()], [X.b(tile)])
            layernorm_tile(k, X, tile, G2, B2, stt_)
            if last:
                fw.dma("sp", dr["y"][tile * 128:(tile + 1) * 128, :], X[:, tile, :], reads=[X.b(tile)], final=True)
            else:
                make_xT(k, tile, layer + 1, False)
        fw.flush()


def layernorm_tile(k, X, tile, Gt, Bt, stt_):
    fw = k.fw
    xs = X[:, tile, :]
    for h in range(2):
        fw.op("dve", lambda e, h=h: e.bn_stats(out=stt_[:, h * 6:(h + 1) * 6], in_=X[:, tile, h * 512:(h + 1) * 512]),
              [X.b(tile)], [stt_.b()])
    fw.op("dve", lambda e: e.bn_aggr(out=stt_[:, 12:14], in_=stt_[:, 0:12]), [stt_.b()], [stt_.b()])
    k.act(stt_[:, 14:15], stt_[:, 13:14], AF.Sqrt, [stt_.b()], [stt_.b()], bias=k.C_eps[:, 0:1])
    fw.op("dve", lambda e: e.reciprocal(out=stt_[:, 15:16], in_=stt_[:, 14:15]), [stt_.b()], [stt_.b()])
    k.ts("dve", xs, xs, stt_[:, 12:13], stt_[:, 15:16], ALU.subtract, ALU.mult, [X.b(tile), stt_.b()], [X.b(tile)])
    k.tt("dve", xs, xs, Gt[:], ALU.mult, [X.b(tile), Gt.b()], [X.b(tile)])
    k.tt("dve", xs, xs, Bt[:], ALU.add, [X.b(tile), Bt.b()], [X.b(tile)])


def load_layer_router(k, layer):
    dr = k.dram
    k.fw.dma("sp", k.B_wrt[:], dr["w_rt"][layer * D:(layer + 1) * D, :].rearrange("(c p) e -> p c e", p=128), writes=[k.B_wrt.b()])
    k.fw.dma("sp", k.B_brt[:], dr["b_rt"][layer:layer + 1, :].partition_broadcast(128), writes=[k.B_brt.b()])


def build(cfg):
    k = K(cfg)
    declare_dram(k)
    if cfg.get("dbg"):
        for n, shp in cfg["dbg"].items():
            k.dout(n, shp)
    with k.st:
        k.setup()
        fw = k.fw
        k.X = k.sb("X", [128, NT, D])
        k.XT = k.sb("XT", [128, 8, SEQ], BF16)
        k.B_xtf = k.sb("xtf", [128, 8, 128])
        k.B_wrt = k.sb("wrt", [128, 8, NEXP])
        k.B_brt = k.sb("brt", [128, NEXP])
        k.B_lg = k.sb("lg", [128, NT, NEXP])
        k.C_eps = k.sb("eps", [128, 4])
        k.vf = k.nc.dram_tensor("vfirst", [W, SEQ], F32, kind="Internal").ap()
        k.vfb = Buf()
        load_consts(k)
        fw.op("dve", lambda e: e.memset(k.C_eps[:, 0:1], LN_EPS), [], [k.C_eps.b()])
        fw.op("dve", lambda e: e.memset(k.C_eps[:, 1:2], NORM_EPS), [], [k.C_eps.b()])
        for tile in range(NT):
            fw.dma("sp", k.X[:, tile, :], k.dram["x"][tile * 128:(tile + 1) * 128, :], writes=[k.X.b(tile)])
        layers = cfg.get("layers", list(range(DEPTH)))
        phases = cfg.get("phases", "AB")
        stop = cfg.get("stop", "")

        def dump():
            for tile in range(NT):
                fw.dma("sp", k.dram["y"][tile * 128:(tile + 1) * 128, :], k.X[:, tile, :], reads=[k.X.b(tile)], final=True)
            fw.flush()
        if stop == "xload":
            dump()
            return k
        if "A" not in phases:
            load_layer_router(k, layers[0])
        for tile in range(cfg.get("nxt", NT)):
            make_xT(k, tile, layers[0], "A" not in phases and stop != "xT0")
        fw.flush()
        if stop.startswith("xT"):
            dump()
            return k
        for li, layer in enumerate(layers):
            last = li == len(layers) - 1
            if "A" in phases:
                phase_a(k, layer)
            if "B" in phases:
                phase_b(k, layer, last)
            else:
                for tile in range(NT):
                    fw.dma("sp", k.dram["y"][tile * 128:(tile + 1) * 128, :], k.X[:, tile, :], reads=[k.X.b(tile)], final=True)
                fw.flush()
    return k


_CACHE = {}


def kernel(**inputs):
    maps = build_inmaps(inputs)
    if "k" not in _CACHE:
        _CACHE["k"] = build({})
    k = _CACHE["k"]
    res = run_bass_kernel_spmd(k.nc, maps, core_ids=list(range(8)))
    out = np.stack([np.asarray(res.results[b]["y"], np.float32) for b in range(8)], axis=0)
    return out


COLP = {}


def _colp_layout():
    off = 0
    spec = [("mu", 14), ("w0", 4), ("a0", 4), ("v0", 4), ("k_k", 4), ("k_a", 4), ("r_k", 4), ("lnx_g", 4), ("lnx_b", 4),
            ("hg_nw", 4), ("ml_cw", 32), ("ml_cb", 8), ("ml_nw", 4), ("lr_cw", 16), ("lr_cb", 4), ("lr_bx", 4), ("lr_ba", 4),
            ("lr_lam", 4), ("hg_lb", 16)]
    for n, c in spec:
        COLP[n] = (off, c)
        off += c
    return off


NCOLP = _colp_layout()
OFF_B = 1792
OFF_C = 3840
OFF_D = 5896
OFF_G = 6920


def host_colp(inputs):
    L = DEPTH
    out = np.zeros((L, 128, NCOLP), np.float32)
    g = lambda n: np.asarray(inputs[n], np.float32)

    def put(l, name, arr2d):
        o, c = COLP[name]
        assert arr2d.shape == (128, c), (name, arr2d.shape)
        out[l, :, o:o + c] = arr2d
    for l in range(L):
        put(l, "mu", _cols(g("rwkv_mu")[l]))
        put(l, "w0", _cols(g("rwkv_w0")[l])); put(l, "a0", _cols(g("rwkv_a0")[l]))
        if l > 0:
            put(l, "v0", _cols(g("rwkv_v0")[l - 1]))
        put(l, "k_k", _cols(g("rwkv_k_k")[l])); put(l, "k_a", _cols(g("rwkv_k_a")[l]))
        put(l, "r_k", _cols(g("rwkv_r_k")[l].reshape(-1)))
        put(l, "lnx_g", _cols(g("rwkv_lnx_g")[l])); put(l, "lnx_b", _cols(g("rwkv_lnx_b")[l]))
        put(l, "hg_nw", _cols(g("hgrn_norm_w")[l]))
        put(l, "ml_cw", np.concatenate([_cols(g("mlstm_conv_w")[l, j]) for j in range(4)], axis=1))
        put(l, "ml_cb", _cols(g("mlstm_conv_b")[l])); put(l, "ml_nw", _cols(g("mlstm_norm_w")[l]))
        put(l, "lr_cw", np.concatenate([_cols(g("lru_conv_w")[l, j]) for j in range(4)], axis=1))
        put(l, "lr_cb", _cols(g("lru_conv_b")[l]))
        put(l, "lr_bx", _cols(g("lru_bx")[l].reshape(-1))); put(l, "lr_ba", _cols(g("lru_ba")[l].reshape(-1)))
        put(l, "lr_lam", _cols(g("lru_lambda")[l]))
        put(l, "hg_lb", np.concatenate([_cols(g("hgrn_lower_bounds")[ll]) for ll in range(L)], axis=1))
    return out.reshape(L * 128, NCOLP)


def build_inmaps_a(inputs, shared):
    f = lambda a: np.ascontiguousarray(np.asarray(a, np.float32))
    L = DEPTH
    shared["colp"] = host_colp(inputs)
    shared["rw_w2"] = f(inputs["rwkv_w2"]).reshape(L * 64, W)
    shared["rw_a2"] = f(inputs["rwkv_a2"]).reshape(L * 64, W)
    shared["rw_v2"] = f(inputs["rwkv_v2"]).reshape(3 * 32, W)
    shared["rw_g2"] = f(inputs["rwkv_g2"]).reshape(L * 128, W)
    shared["lr_wx"] = f(inputs["lru_wx"]).reshape(L * 8 * 64, 64)
    shared["lr_wa"] = f(inputs["lru_wa"]).reshape(L * 8 * 64, 64)
    shared["ml_ib"] = f(inputs["mlstm_i_bias"])
    shared["ml_fb"] = f(inputs["mlstm_f_bias"])


def declare_dram_a(k):
    L = DEPTH
    k.din("colp", [L * 128, NCOLP])
    k.din("rw_w2", [L * 64, W]); k.din("rw_a2", [L * 64, W]); k.din("rw_v2", [3 * 32, W]); k.din("rw_g2", [L * 128, W])
    k.din("lr_wx", [L * 8 * 64, 64]); k.din("lr_wa", [L * 8 * 64, 64])
    k.din("ml_ib", [L, 4]); k.din("ml_fb", [L, 4])


def w_in_ap(k, layer, c0, nc_):
    if layer == 0:
        return k.dram["w_in_first"][:, c0:c0 + nc_]
    return k.dram["w_in_rest"][(layer - 1) * D:layer * D, c0:c0 + nc_]


def load_w_multi(k, pieces, kc=8):
    tot = sum(int(p.shape[1]) for p in pieces)
    s = wslot(k)
    full = k.ring[s]
    view = full[:, 0:kc * tot].rearrange("p (a c) -> p a c", a=kc)
    o = 0
    for p in pieces:
        n = int(p.shape[1])
        k.fw.dma("pool", view[:, :, o:o + n], p.rearrange("(a p) c -> p a c", p=128), writes=[k.ringb[s]])
        o += n
    return view, k.ringb[s]


def zt_mm(k, wview, wbuf, c0, ncols, tb, M=None):
    p = k.ps()
    ts_ = slice(tb * TBA, (tb + 1) * TBA)
    xb = [k.XT.b(tb * 2 + i) for i in range(2)]
    for c in range(8):
        k.mm(p[0:ncols, 0:TBA], wview[:, c, c0:c0 + ncols], k.XT[:, c, ts_], c == 0, c == 7, xb + [wbuf], [p.b()])
    return p


def cp_(k, name, j=0):
    o, c = COLP[name]
    return k.A["colp"][:, o + j:o + j + 1]


def phase_a(k, layer):
    fw = k.fw
    nc = k.nc
    dr = k.dram
    st = contextlib.ExitStack()
    mixers = k.cfg.get("mixers", "ABCD")
    with st:
        A = {}
        k.A = A
        k.NRING = 3
        k.ring = []
        k.ringb = []
        for i in range(k.NRING):
            k.ring.append(st.enter_context(nc.sbuf_tensor("ringA%d_%d" % (layer, i), [128, 4096], BF16)))
            k.ringb.append(Buf())
        k.ring_rr = 0
        A["colp"] = k.sb("colp%d" % layer, [128, NCOLP], F32, st)
        fw.dma("sp", A["colp"][:], dr["colp"][layer * 128:(layer + 1) * 128, :], writes=[A["colp"].b()])
        A["YT"] = [k.sb("YT%d_%d" % (layer, n), [128, 4, TBA], BF16, st) for n in range(4)]
        A["MT"] = k.sb("MT%d" % layer, [128, 8, TBA], BF16, st)
        A["G1"] = k.sb("G1_%d" % layer, [128, D], F32, st)
        A["B1"] = k.sb("B1_%d" % layer, [128, D], F32, st)
        fw.dma("sp", A["G1"][:], dr["ln1_g"][layer:layer + 1, :].partition_broadcast(128), writes=[A["G1"].b()])
        fw.dma("sp", A["B1"][:], dr["ln1_b"][layer:layer + 1, :].partition_broadcast(128), writes=[A["B1"].b()])
        NS = k.cfg.get("NS", 16)
        A["S"] = [k.sb("S%d_%d" % (layer, i), [128, 520], F32, st) for i in range(NS)]
        A["sm"] = k.sb("smA%d" % layer, [128, 64], F32, st)
        A["stt"] = k.sb("sttA%d" % layer, [128, 16], F32, st)
        load_layer_router(k, layer)
        for n in range(4):
            if "ABCD"[n] not in mixers:
                fw.op("pool", lambda e, n=n: e.memset(A["YT"][n][:], 0.0), [], [A["YT"][n].b()])
        if "D" in mixers:
            lru_setup(k, layer, st)
        if "C" in mixers:
            mlstm_setup(k, layer, st)
        if "B" in mixers:
            hgrn_setup(k, layer, st)
        if "A" in mixers:
            rwkv_setup(k, layer, st)
        for tb in range(k.cfg.get("ntb", NBA)):
            if "D" in mixers:
                lru_block(k, layer, tb)
            if "C" in mixers:
                mlstm_block(k, layer, tb)
            if "B" in mixers:
                hgrn_block(k, layer, tb)
            if "A" in mixers:
                rwkv_block(k, layer, tb)
            if k.cfg.get("dbgY"):
                for n in range(4):
                    for j in range(4):
                        fw.dma("pool", dr["dbg_y"][n * W + j * 128:n * W + (j + 1) * 128, tb * TBA:(tb + 1) * TBA],
                               A["YT"][n][:, j, :], reads=[A["YT"][n].b()], final=True)
            if not k.cfg.get("nomerge"):
                merge_block(k, layer, tb)
        fw.flush()


def lru_setup(k, layer, st):
    fw = k.fw
    A = k.A
    dr = k.dram
    A["lr_wx"] = k.sb("lrwx%d" % layer, [128, 4, 128], F32, st)
    A["lr_wa"] = k.sb("lrwa%d" % layer, [128, 4, 128], F32, st)
    for nm, src in (("lr_wx", "lr_wx"), ("lr_wa", "lr_wa")):
        t = A[nm]
        fw.op("dve", lambda e, t=t: e.memset(t[:], 0.0), [], [t.b()])
        for g in range(8):
            j, h = g // 2, g % 2
            r0 = (layer * 8 + g) * 64
            fw.dma("sp", t[h * 64:(h + 1) * 64, j, h * 64:(h + 1) * 64], dr[src][r0:r0 + 64, :], writes=[t.b()])
    A["lr_c8"] = k.sb("lrc8_%d" % layer, [128, 8], F32, st)
    c8 = A["lr_c8"]
    o, _ = COLP["lr_lam"]
    k.act(c8[:, 0:4], A["colp"][:, o:o + 4], AF.Sigmoid, [A["colp"].b()], [c8.b()])
    k.act(c8[:, 0:4], c8[:, 0:4], AF.Ln, [c8.b()], [c8.b()])
    k.ts("dve", c8[:, 4:8], c8[:, 0:4], 16.0, None, ALU.mult, None, [c8.b()], [c8.b()])
    k.ts("dve", c8[:, 0:4], c8[:, 0:4], 8.0, None, ALU.mult, None, [c8.b()], [c8.b()])
    A["lr_halo"] = k.sb("lrhalo%d" % layer, [128, 4, 4], F32, st)
    A["lr_hc"] = k.sb("lrhc%d" % layer, [128, 4], F32, st)
    fw.op("dve", lambda e: e.memset(A["lr_halo"][:], 0.0), [], [A["lr_halo"].b()])
    fw.op("dve", lambda e: e.memset(A["lr_hc"][:], 0.0), [], [A["lr_hc"].b()])


def conv4(k, zt, acc, wname, bname, j, nchunks):
    o, _ = COLP[wname]
    cw = lambda i: k.A["colp"][:, o + i * nchunks + j:o + i * nchunks + j + 1]
    cb = cp_(k, bname, j)
    rb = [zt.b(), k.A["colp"].b()]
    k.ts("dve", acc[:, 0:TBA], zt[:, 3:3 + TBA], cw(3), cb, ALU.mult, ALU.add, rb, [acc.b()])
    for i in (2, 1, 0):
        k.stt(acc[:, 0:TBA], zt[:, i:i + TBA], cw(i), acc[:, 0:TBA], ALU.mult, ALU.add, rb + [acc.b()], [acc.b()])


def lru_block(k, layer, tb):
    fw = k.fw
    A = k.A
    S = A["S"]
    YT = A["YT"][3]
    wx, bx_ = load_w(k, w_in_ap(k, layer, OFF_D, 512), 8, 512)
    wg, bg_ = load_w(k, w_in_ap(k, layer, OFF_D + 512, 512), 8, 512)
    halo, hc, c8 = A["lr_halo"], A["lr_hc"], A["lr_c8"]
    for j in range(4):
        zt, xc, ga, gx, t1, u = S[0], S[1], S[2], S[3], S[4], S[5]
        p = zt_mm(k, wx, bx_, j * 128, 128, tb)
        k.cp("dve", zt[:, 0:3], halo[:, j, 0:3], [halo.b()], [zt.b()])
        k.cp("act", zt[:, 3:3 + TBA], p[:, 0:TBA], [p.b()], [zt.b()])
        k.cp("dve", halo[:, j, 0:3], zt[:, TBA:TBA + 3], [zt.b()], [halo.b()])
        conv4(k, zt, xc, "lr_cw", "lr_cb", j, 4)
        pa = k.ps()
        k.mm(pa[:, 0:TBA], A["lr_wa"][:, j, :], xc[:, 0:TBA], True, True, [A["lr_wa"].b(), xc.b()], [pa.b()])
        px = k.ps()
        k.mm(px[:, 0:TBA], A["lr_wx"][:, j, :], xc[:, 0:TBA], True, True, [A["lr_wx"].b(), xc.b()], [px.b()])
        k.act(ga[:, 0:TBA], pa[:, 0:TBA], AF.Sigmoid, [pa.b(), A["colp"].b()], [ga.b()], bias=cp_(k, "lr_ba", j))
        k.act(gx[:, 0:TBA], px[:, 0:TBA], AF.Sigmoid, [px.b(), A["colp"].b()], [gx.b()], bias=cp_(k, "lr_bx", j))
        k.ts("dve", ga[:, 0:TBA], ga[:, 0:TBA], c8[:, j:j + 1], None, ALU.mult, None, [ga.b(), c8.b()], [ga.b()])
        k.act(ga[:, 0:TBA], ga[:, 0:TBA], AF.Exp, [ga.b()], [ga.b()])
        k.tt("dve", t1[:, 0:TBA], ga[:, 0:TBA], ga[:, 0:TBA], ALU.mult, [ga.b()], [t1.b()])
        k.ts("dve", t1[:, 0:TBA], t1[:, 0:TBA], -1.0, 1.0, ALU.mult, ALU.add, [t1.b()], [t1.b()])
        k.act(t1[:, 0:TBA], t1[:, 0:TBA], AF.Sqrt, [t1.b()], [t1.b()])
        if tb == 0:
            fw.op("dve", lambda e, t1=t1: e.memset(t1[:, 0:1], 1.0), [], [t1.b()])
        k.tt("dve", gx[:, 0:TBA], gx[:, 0:TBA], xc[:, 0:TBA], ALU.mult, [gx.b(), xc.b()], [gx.b()])
        k.tt("dve", gx[:, 0:TBA], gx[:, 0:TBA], t1[:, 0:TBA], ALU.mult, [gx.b(), t1.b()], [gx.b()])
        fw.op("dve", lambda e, ga=ga, gx=gx, xc=xc, j=j: e.tensor_tensor_scan(
            out=xc[:, 0:TBA], data0=ga[:, 0:TBA], data1=gx[:, 0:TBA], initial=hc[:, j:j + 1], op0=ALU.mult, op1=ALU.add),
            [ga.b(), gx.b(), hc.b()], [xc.b()])
        k.cp("dve", hc[:, j:j + 1], xc[:, TBA - 1:TBA], [xc.b()], [hc.b()])
        pg = zt_mm(k, wg, bg_, j * 128, 128, tb)
        k.cp("act", u[:, 0:TBA], pg[:, 0:TBA], [pg.b()], [u.b()])
        k.tt("dve", t1[:, 0:TBA], u[:, 0:TBA], u[:, 0:TBA], ALU.mult, [u.b()], [t1.b()])
        k.ts("dve", t1[:, 0:TBA], t1[:, 0:TBA], 0.044715, 1.0, ALU.mult, ALU.add, [t1.b()], [t1.b()])
        k.tt("dve", t1[:, 0:TBA], t1[:, 0:TBA], u[:, 0:TBA], ALU.mult, [t1.b(), u.b()], [t1.b()])
        k.act(t1[:, 0:TBA], t1[:, 0:TBA], AF.Sigmoid, [t1.b()], [t1.b()], scale=1.5957691216057308)
        k.tt("dve", t1[:, 0:TBA], t1[:, 0:TBA], u[:, 0:TBA], ALU.mult, [t1.b(), u.b()], [t1.b()])
        k.tt("dve", YT[:, j, :], t1[:, 0:TBA], xc[:, 0:TBA], ALU.mult, [t1.b(), xc.b()], [YT.b()])


def merge_block(k, layer, tb):
    fw = k.fw
    A = k.A
    S = A["S"]
    dr = k.dram
    MT = A["MT"]
    X = k.X
    for c in range(8):
        wg, bg_ = load_w_multi(k, [w_in_ap(k, layer, OFF_G + n * D + c * 128, 128) for n in range(4)], 8)
        wb, bb_ = load_w_multi(k, [dr["w_br"][(layer * 4 + n) * W:(layer * 4 + n + 1) * W, c * 128:(c + 1) * 128] for n in range(4)], 4)
        acc = S[0]
        for n in range(4):
            pz = zt_mm(k, wg, bg_, n * 128, 128, tb)
            sg = S[1 + (n % 2)]
            k.act(sg[:, 0:TBA], pz[:, 0:TBA], AF.Sigmoid, [pz.b()], [sg.b()])
            pp = k.ps()
            for kc in range(4):
                k.mm(pp[:, 0:TBA], wb[:, kc, n * 128:(n + 1) * 128], A["YT"][n][:, kc, :], kc == 0, kc == 3,
                     [bb_, A["YT"][n].b()], [pp.b()])
            if n == 0:
                k.tt("dve", acc[:, 0:TBA], sg[:, 0:TBA], pp[:, 0:TBA], ALU.mult, [sg.b(), pp.b()], [acc.b()])
            else:
                k.tt("dve", sg[:, 0:TBA], sg[:, 0:TBA], pp[:, 0:TBA], ALU.mult, [sg.b(), pp.b()], [sg.b()])
                if n < 3:
                    k.tt("dve", acc[:, 0:TBA], acc[:, 0:TBA], sg[:, 0:TBA], ALU.add, [acc.b(), sg.b()], [acc.b()])
                else:
                    k.tt("dve", MT[:, c, :], acc[:, 0:TBA], sg[:, 0:TBA], ALU.add, [acc.b(), sg.b()], [MT.b()])
    wo = [load_w(k, dr["w_out"][layer * D:(layer + 1) * D, h * 512:(h + 1) * 512], 8, 512) for h in range(2)]
    for i in range(TBA // 128):
        tile = tb * (TBA // 128) + i
        for h in range(2):
            cs = slice(h * 512, (h + 1) * 512)
            p = k.ps()
            for kc in range(8):
                k.mm(p[:, :], MT[:, kc, i * 128:(i + 1) * 128], wo[h][0][:, kc, :], kc == 0, kc == 7, [MT.b(), wo[h][1]], [p.b()])
            k.stt(X[:, tile, cs], X[:, tile, cs], ALPHA, p[:, :], ALU.mult, ALU.add, [X.b(tile), p.b()], [X.b(tile)])
        layernorm_tile(k, X, tile, A["G1"], A["B1"], A["stt"])
        make_xT(k, tile, layer, True)


def v3(ap2d, c):
    return ap2d.rearrange("p (c t) -> p c t", c=c)


def hgrn_setup(k, layer, st):
    fw = k.fw
    A = k.A
    A["hg_S"] = [k.sb("hgS%d_%d" % (layer, h), [128, 128], F32, st) for h in range(4)]
    for h in range(4):
        fw.op("pool", lambda e, h=h: e.memset(A["hg_S"][h][:], 0.0), [], [A["hg_S"][h].b()])
    lbt = k.sb("hglb%d" % layer, [128, 32], F32, st)
    A["hg_lbt"] = lbt
    o, _ = COLP["hg_lb"]
    rb = [A["colp"].b()]
    E = lbt[:, 0:16]
    k.act(E, A["colp"][:, o:o + 16], AF.Exp, rb, [lbt.b()])
    tot, num = lbt[:, 16:20], lbt[:, 20:24]
    k.tt("dve", tot, lbt[:, 0:4], lbt[:, 4:8], ALU.add, [lbt.b()], [lbt.b()])
    k.tt("dve", tot, tot, lbt[:, 8:12], ALU.add, [lbt.b()], [lbt.b()])
    k.tt("dve", tot, tot, lbt[:, 12:16], ALU.add, [lbt.b()], [lbt.b()])
    fw.op("dve", lambda e: e.memset(num, 0.0), [], [lbt.b()])
    for ll in range(1, layer + 1):
        k.tt("dve", num, num, lbt[:, ll * 4:(ll + 1) * 4], ALU.add, [lbt.b()], [lbt.b()])
    fw.op("dve", lambda e: e.reciprocal(out=tot, in_=tot), [lbt.b()], [lbt.b()])
    k.tt("dve", lbt[:, 24:28], num, tot, ALU.mult, [lbt.b()], [lbt.b()])
    k.ts("dve", lbt[:, 28:32], lbt[:, 24:28], -1.0, 1.0, ALU.mult, ALU.add, [lbt.b()], [lbt.b()])


def hgrn_block(k, layer, tb):
    fw = k.fw
    A = k.A
    S = A["S"]
    C = k.C
    sm = A["sm"]
    lbt = A["hg_lbt"]
    YT = A["YT"][1]
    NCH = TBA // 64
    ident = C["ident"]
    for h in range(4):
        wv, wb_ = load_w_multi(k, [w_in_ap(k, layer, OFF_B + q * W + h * 128, 128) for q in range(4)], 8)
        qt, bc, kt, eq, ek, vT, sgt, Kt, Vt, AT, O, sq, Sp = S[0:13]
        St = A["hg_S"][h]
        pq = zt_mm(k, wv, wb_, 0, 128, tb)
        k.act(qt[:, 0:TBA], pq[:, 0:TBA], AF.Silu, [pq.b()], [qt.b()])
        pf = zt_mm(k, wv, wb_, 128, 128, tb)
        k.act(kt[:, 0:TBA], pf[:, 0:TBA], AF.Sigmoid, [pf.b()], [kt.b()])
        k.ts("dve", kt[:, 0:TBA], kt[:, 0:TBA], lbt[:, 28 + h:29 + h], lbt[:, 24 + h:25 + h], ALU.mult, ALU.add, [kt.b(), lbt.b()], [kt.b()])
        k.act(bc[:, 0:TBA], kt[:, 0:TBA], AF.Ln, [kt.b()], [bc.b()])
        k.ts("dve", kt[:, 0:TBA], kt[:, 0:TBA], -1.0, 1.0, ALU.mult, ALU.add, [kt.b()], [kt.b()])
        fw.op("dve", lambda e, bc=bc: e.tensor_tensor_scan(out=bc[:, 0:TBA], data0=C["rmask"][:, 0:TBA], data1=bc[:, 0:TBA],
                                                            initial=0.0, op0=ALU.mult, op1=ALU.add), [bc.b(), C["rmask"].b()], [bc.b()])
        bc3 = v3(bc[:, 0:TBA], NCH)
        k.act(sm[:, 0:NCH], bc3[:, :, 63], AF.Exp, [bc.b()], [sm.b()])
        k.act(sm[:, 8:8 + NCH], bc3[:, :, 31], AF.Exp, [bc.b()], [sm.b()])
        k.cp("dve", sm[:, 16:16 + NCH], bc3[:, :, 31], [bc.b()], [sm.b()])
        k.tt("dve", bc3, bc3, v3(sm[:, 16:16 + NCH], NCH).broadcast_to([128, NCH, 64]), ALU.subtract, [bc.b(), sm.b()], [bc.b()])
        k.act(sm[:, 4:4 + NCH], bc3[:, :, 63], AF.Exp, [bc.b()], [sm.b()])
        k.act(eq[:, 0:TBA], bc[:, 0:TBA], AF.Exp, [bc.b()], [eq.b()])
        k.act(ek[:, 0:TBA], bc[:, 0:TBA], AF.Exp, [bc.b()], [ek.b()], scale=-1.0)
        k.tt("dve", qt[:, 0:TBA], qt[:, 0:TBA], eq[:, 0:TBA], ALU.mult, [qt.b(), eq.b()], [qt.b()])
        k.tt("dve", kt[:, 0:TBA], kt[:, 0:TBA], ek[:, 0:TBA], ALU.mult, [kt.b(), ek.b()], [kt.b()])
        pv = zt_mm(k, wv, wb_, 256, 128, tb)
        k.cp("act", vT[:, 0:TBA], pv[:, 0:TBA], [pv.b()], [vT.b()])
        pg = zt_mm(k, wv, wb_, 384, 128, tb)
        k.act(sgt[:, 0:TBA], pg[:, 0:TBA], AF.Silu, [pg.b()], [sgt.b()])
        for src, dst in ((kt, Kt), (vT, Vt)):
            p = k.ps()
            for c in range(NCH):
                k.tr(p[0:64, c * 128:(c + 1) * 128], src[:, c * 64:(c + 1) * 64], ident[:], [src.b(), ident.b()], [p.b()])
            k.cp("act", dst[0:64, 0:NCH * 128], p[0:64, 0:NCH * 128], [p.b()], [dst.b()])
        pA = k.ps()
        for c in range(NCH):
            cs = slice(c * 64, (c + 1) * 64)
            k.mm(pA[0:64, cs], kt[:, cs], qt[:, cs], True, True, [kt.b(), qt.b()], [pA.b()])
        k.tt("dve", AT[0:64, 0:TBA], pA[0:64, 0:TBA], C["m_incl"][0:64, 0:TBA], ALU.mult, [pA.b(), C["m_incl"].b()], [AT.b()])
        pKV = k.ps()
        for c in range(NCH):
            k.mm(pKV[:, c * 128:(c + 1) * 128], Kt[0:64, c * 128:(c + 1) * 128], Vt[0:64, c * 128:(c + 1) * 128], True, True,
                 [Kt.b(), Vt.b()], [pKV.b()])
        for c in range(NCH):
            cs = slice(c * 64, (c + 1) * 64)
            k.ts("dve", Sp[:, 0:128], St[:], sm[:, 8 + c:9 + c], None, ALU.mult, None, [St.b(), sm.b()], [Sp.b()])
            po = k.ps()
            k.mm(po[0:64, 0:128], qt[:, cs], Sp[:, 0:128], True, False, [qt.b(), Sp.b()], [po.b()])
            k.mm(po[0:64, 0:128], AT[0:64, cs], Vt[0:64, c * 128:(c + 1) * 128], False, True, [AT.b(), Vt.b()], [po.b()])
            k.cp("act", O[0:64, c * 128:(c + 1) * 128], po[0:64, 0:128], [po.b()], [O.b()])
            k.ts("dve", St[:], St[:], sm[:, c:c + 1], None, ALU.mult, None, [St.b(), sm.b()], [St.b()])
            k.stt(St[:], pKV[:, c * 128:(c + 1) * 128], sm[:, 4 + c:5 + c], St[:], ALU.mult, ALU.add, [pKV.b(), sm.b(), St.b()], [St.b()])
        O3 = v3(O[0:64, 0:NCH * 128], NCH)
        k.tt("dve", sq[0:64, 0:NCH * 128], O[0:64, 0:NCH * 128], O[0:64, 0:NCH * 128], ALU.mult, [O.b()], [sq.b()])
        fw.op("dve", lambda e, sq=sq: e.tensor_reduce(out=sm[0:64, 24:24 + NCH], in_=v3(sq[0:64, 0:NCH * 128], NCH), axis=AX.X, op=ALU.add),
              [sq.b()], [sm.b()])
        k.ts("dve", sm[0:64, 24:24 + NCH], sm[0:64, 24:24 + NCH], 1.0 / 128, NORM_EPS, ALU.mult, ALU.add, [sm.b()], [sm.b()])
        k.act(sm[0:64, 24:24 + NCH], sm[0:64, 24:24 + NCH], AF.Sqrt, [sm.b()], [sm.b()])
        fw.op("dve", lambda e: e.reciprocal(out=sm[0:64, 28:28 + NCH], in_=sm[0:64, 24:24 + NCH]), [sm.b()], [sm.b()])
        k.tt("dve", O3, O3, v3(sm[0:64, 28:28 + NCH], NCH).broadcast_to([64, NCH, 128]), ALU.mult, [O.b(), sm.b()], [O.b()])
        pT = k.ps()
        for c in range(NCH):
            k.tr(pT[:, c * 64:(c + 1) * 64], O[0:64, c * 128:(c + 1) * 128], ident[0:64, 0:64], [O.b(), ident.b()], [pT.b()])
        k.stt(YT[:, h, :], pT[:, 0:TBA], cp_(k, "hg_nw", h), sgt[:, 0:TBA], ALU.mult, ALU.mult, [pT.b(), A["colp"].b(), sgt.b()], [YT.b()])


def mlstm_setup(k, layer, st):
    fw = k.fw
    A = k.A
    dr = k.dram
    A["ml_C"] = [k.sb("mlC%d_%d" % (layer, h), [128, 132], F32, st) for h in range(4)]
    for h in range(4):
        fw.op("pool", lambda e, h=h: e.memset(A["ml_C"][h][:], 0.0), [], [A["ml_C"][h].b()])
    A["ml_halo"] = k.sb("mlhalo%d" % layer, [128, 8, 4], F32, st)
    fw.op("pool", lambda e: e.memset(A["ml_halo"][:], 0.0), [], [A["ml_halo"].b()])
    A["ml_g"] = k.sb("mlg%d" % layer, [64, 112], F32, st)
    A["ml_efl"] = k.sb("mlefl%d" % layer, [128, 16], F32, st)
    g = A["ml_g"]
    fw.dma("sp", g[:, 96:100], dr["ml_ib"][layer:layer + 1, :].partition_broadcast(64), writes=[g.b()])
    fw.dma("sp", g[:, 100:104], dr["ml_fb"][layer:layer + 1, :].partition_broadcast(64), writes=[g.b()])


def mlstm_block(k, layer, tb):
    fw = k.fw
    A = k.A
    S = A["S"]
    C = k.C
    sm = A["sm"]
    YT = A["YT"][2]
    NCH = TBA // 64
    ident = C["ident"]
    g = A["ml_g"]
    efl = A["ml_efl"]
    XT = k.XT
    wgv, wgb = load_w(k, w_in_ap(k, layer, OFF_C + 2048, 8), 8, 8)
    pgt = k.ps()
    for c in range(NCH):
        t0 = tb * TBA + c * 64
        tl = t0 // 128
        for kc in range(8):
            k.mm(pgt[0:64, c * 8:(c + 1) * 8], XT[:, kc, t0:t0 + 64], wgv[:, kc, :], kc == 0, kc == 7, [XT.b(tl), wgb], [pgt.b()])
    GT3 = v3(g[:, 0:32], NCH)
    k.tt("dve", GT3, v3(pgt[0:64, 0:32], NCH), g[:, 96:104].unsqueeze(1).broadcast_to([64, NCH, 8]), ALU.add, [pgt.b(), g.b()], [g.b()])
    LF3 = v3(g[:, 32:48], NCH)
    k.act(LF3, GT3[:, :, 4:8], AF.Sigmoid, [g.b()], [g.b()])
    k.act(g[:, 32:48], g[:, 32:48], AF.Ln, [g.b()], [g.b()])
    pF = k.ps()
    k.mm(pF[0:64, 0:16], C["m_incl"][0:64, 0:64], g[:, 32:48], True, True, [C["m_incl"].b(), g.b()], [pF.b()])
    pL = k.ps()
    k.mm(pL[:, 0:16], C["ones"][0:64, :], g[:, 32:48], True, True, [C["ones"].b(), g.b()], [pL.b()])
    k.act(efl[:, 0:16], pL[:, 0:16], AF.Exp, [pL.b()], [efl.b()])
    k.tt("dve", v3(g[:, 64:80], NCH), GT3[:, :, 0:4], v3(pF[0:64, 0:16], NCH), ALU.subtract, [g.b(), pF.b()], [g.b()])
    k.act(g[:, 64:80], g[:, 64:80], AF.Exp, [g.b()], [g.b()])
    k.act(g[:, 80:96], pF[0:64, 0:16], AF.Exp, [pF.b()], [g.b()])
    halo = A["ml_halo"]
    for h in range(4):
        wv, wb_ = load_w_multi(k, [w_in_ap(k, layer, OFF_C + q * W + h * 128, 128) for q in range(4)], 8)
        zt, qT, kT, vT, so, Kg, Vx, WT, NUM, sq = S[0:10]
        Ct = A["ml_C"][h]
        for qi, dst in ((0, qT), (1, kT)):
            hj = qi * 4 + h
            p = zt_mm(k, wv, wb_, qi * 128, 128, tb)
            k.cp("dve", zt[:, 0:3], halo[:, hj, 0:3], [halo.b()], [zt.b()])
            k.cp("act", zt[:, 3:3 + TBA], p[:, 0:TBA], [p.b()], [zt.b()])
            k.cp("dve", halo[:, hj, 0:3], zt[:, TBA:TBA + 3], [zt.b()], [halo.b()])
            conv4(k, zt, dst, "ml_cw", "ml_cb", hj, 8)
            k.act(dst[:, 0:TBA], dst[:, 0:TBA], AF.Silu, [dst.b()], [dst.b()])
        k.ts("dve", kT[:, 0:TBA], kT[:, 0:TBA], float(128 ** -0.5), None, ALU.mult, None, [kT.b()], [kT.b()])
        pv = zt_mm(k, wv, wb_, 256, 128, tb)
        k.cp("act", vT[:, 0:TBA], pv[:, 0:TBA], [pv.b()], [vT.b()])
        po_ = zt_mm(k, wv, wb_, 384, 128, tb)
        k.act(so[:, 0:TBA], po_[:, 0:TBA], AF.Sigmoid, [po_.b()], [so.b()])
        p = k.ps()
        for c in range(NCH):
            k.tr(p[0:64, c * 128:(c + 1) * 128], kT[:, c * 64:(c + 1) * 64], ident[:], [kT.b(), ident.b()], [p.b()])
        for c in range(NCH):
            k.ts("dve", Kg[0:64, c * 128:(c + 1) * 128], p[0:64, c * 128:(c + 1) * 128], g[:, 64 + c * 4 + h:65 + c * 4 + h], None,
                 ALU.mult, None, [p.b(), g.b()], [Kg.b()])
        p = k.ps()
        for c in range(NCH):
            k.tr(p[0:64, c * 128:(c + 1) * 128], vT[:, c * 64:(c + 1) * 64], ident[:], [vT.b(), ident.b()], [p.b()])
        Vx3 = v3(Vx[0:64, 0:NCH * 129], NCH)
        k.cp("act", Vx3[:, :, 0:128], v3(p[0:64, 0:NCH * 128], NCH), [p.b()], [Vx.b()])
        fw.op("dve", lambda e, Vx3=Vx3: e.memset(Vx3[:, :, 128:129], 1.0), [], [Vx.b()])
        pA = k.ps()
        for c in range(NCH):
            cs = slice(c * 64, (c + 1) * 64)
            k.mm(pA[0:64, cs], kT[:, cs], qT[:, cs], True, True, [kT.b(), qT.b()], [pA.b()])
        for c in range(NCH):
            cs = slice(c * 64, (c + 1) * 64)
            k.stt(WT[0:64, cs], pA[0:64, cs], g[:, 64 + c * 4 + h:65 + c * 4 + h], C["m_incl"][0:64, 0:64], ALU.mult, ALU.mult,
                  [pA.b(), g.b(), C["m_incl"].b()], [WT.b()])
        pKV = [k.ps(), k.ps()]
        for c in range(NCH):
            pk = pKV[c // 2]
            o = (c % 2) * 132
            k.mm(pk[:, o:o + 129], Kg[0:64, c * 128:(c + 1) * 128], Vx3[:, c, :], True, True, [Kg.b(), Vx.b()], [pk.b()])
        NUM3 = v3(NUM[0:64, 0:NCH * 129], NCH)
        for c in range(NCH):
            cs = slice(c * 64, (c + 1) * 64)
            pn = k.ps()
            k.mm(pn[0:64, 0:129], qT[:, cs], Ct[:, 0:129], True, False, [qT.b(), Ct.b()], [pn.b()])
            k.mm(pn[0:64, 0:129], WT[0:64, cs], Vx3[:, c, :], False, True, [WT.b(), Vx.b()], [pn.b()])
            k.cp("act", NUM3[:, c, :], pn[0:64, 0:129], [pn.b()], [NUM.b()])
            ef = efl[:, c * 4 + h:c * 4 + h + 1]
            k.ts("dve", Ct[:, 0:129], Ct[:, 0:129], ef, None, ALU.mult, None, [Ct.b(), efl.b()], [Ct.b()])
            pk = pKV[c // 2]
            o = (c % 2) * 132
            k.stt(Ct[:, 0:129], pk[:, o:o + 129], ef, Ct[:, 0:129], ALU.mult, ALU.add, [pk.b(), efl.b(), Ct.b()], [Ct.b()])
        EFh = v3(g[:, 80:96], NCH)[:, :, h]
        d = sm[0:64, 32:32 + NCH]
        k.tt("dve", d, NUM3[:, :, 128], EFh, ALU.mult, [NUM.b(), g.b()], [sm.b()])
        d2 = sm[0:64, 52:52 + NCH]
        k.ts("dve", d2, d, -1.0, None, ALU.mult, None, [sm.b()], [sm.b()])
        k.tt("dve", d, d, d2, ALU.max, [sm.b()], [sm.b()])
        k.ts("dve", d, d, 1.0, None, ALU.max, None, [sm.b()], [sm.b()])
        fw.op("dve", lambda e, d=d: e.reciprocal(out=d, in_=d), [sm.b()], [sm.b()])
        k.tt("dve", d, d, EFh, ALU.mult, [sm.b(), g.b()], [sm.b()])
        H3 = NUM3[:, :, 0:128]
        bsc = lambda a: v3(a, NCH).broadcast_to([64, NCH, 128])
        k.tt("dve", H3, H3, bsc(d), ALU.mult, [NUM.b(), sm.b()], [NUM.b()])
        s1, s2, mean, rstd = sm[0:64, 36:40], sm[0:64, 40:44], sm[0:64, 44:48], sm[0:64, 48:52]
        fw.op("dve", lambda e, H3=H3, s1=s1: e.tensor_reduce(out=s1, in_=H3, axis=AX.X, op=ALU.add), [NUM.b()], [sm.b()])
        sq3 = v3(sq[0:64, 0:NCH * 128], NCH)
        k.tt("dve", sq3, H3, H3, ALU.mult, [NUM.b()], [sq.b()])
        fw.op("dve", lambda e, sq3=sq3, s2=s2: e.tensor_reduce(out=s2, in_=sq3, axis=AX.X, op=ALU.add), [sq.b()], [sm.b()])
        k.ts("dve", mean, s1, 1.0 / 128, None, ALU.mult, None, [sm.b()], [sm.b()])
        k.tt("dve", s1, mean, mean, ALU.mult, [sm.b()], [sm.b()])
        k.ts("dve", s2, s2, 1.0 / 128, NORM_EPS, ALU.mult, ALU.add, [sm.b()], [sm.b()])
        k.tt("dve", s2, s2, s1, ALU.subtract, [sm.b()], [sm.b()])
        k.act(s2, s2, AF.Sqrt, [sm.b()], [sm.b()])
        fw.op("dve", lambda e, s2=s2, rstd=rstd: e.reciprocal(out=rstd, in_=s2), [sm.b()], [sm.b()])
        k.tt("dve", H3, H3, bsc(mean), ALU.subtract, [NUM.b(), sm.b()], [NUM.b()])
        k.tt("dve", sq3, H3, bsc(rstd), ALU.mult, [NUM.b(), sm.b()], [sq.b()])
        pT = k.ps()
        for c in range(NCH):
            k.tr(pT[:, c * 64:(c + 1) * 64], sq[0:64, c * 128:(c + 1) * 128], ident[0:64, 0:64], [sq.b(), ident.b()], [pT.b()])
        k.stt(YT[:, h, :], pT[:, 0:TBA], cp_(k, "ml_nw", h), so[:, 0:TBA], ALU.mult, ALU.mult, [pT.b(), A["colp"].b(), so.b()], [YT.b()])


def rwkv_setup(k, layer, st):
    fw = k.fw
    A = k.A
    dr = k.dram
    A["rw_ST"] = [k.sb("rwST%d_%d" % (layer, j), [128, 128], F32, st) for j in range(4)]
    for j in range(4):
        fw.op("pool", lambda e, j=j: e.memset(A["rw_ST"][j][:], 0.0), [], [A["rw_ST"][j].b()])
    A["rw_halo"] = k.sb("rwhalo%d" % layer, [128, 16], F32, st)
    fw.op("pool", lambda e: e.memset(A["rw_halo"][:], 0.0), [], [A["rw_halo"].b()])
    wa = k.sb("rwwa%d" % layer, [128, W], F32, st)
    A["rw_wa"] = wa
    fw.dma("sp", wa[0:64, :], dr["rw_w2"][layer * 64:(layer + 1) * 64, :], writes=[wa.b()])
    fw.dma("sp", wa[64:128, :], dr["rw_a2"][layer * 64:(layer + 1) * 64, :], writes=[wa.b()])
    g2 = k.sb("rwg2%d" % layer, [128, W], F32, st)
    A["rw_g2"] = g2
    fw.dma("sp", g2[:], dr["rw_g2"][layer * 128:(layer + 1) * 128, :], writes=[g2.b()])
    if layer > 0:
        v2 = k.sb("rwv2%d" % layer, [32, W], F32, st)
        A["rw_v2"] = v2
        fw.dma("sp", v2[:], dr["rw_v2"][(layer - 1) * 32:layer * 32, :], writes=[v2.b()])
    c = k.sb("rwc%d" % layer, [128, 8], F32, st)
    A["rw_c"] = c
    o, _ = COLP["k_a"]
    k.ts("dve", c[:, 0:4], A["colp"][:, o:o + 4], -1.0, 1.0, ALU.mult, ALU.add, [A["colp"].b()], [c.b()])


def rwkv_block(k, layer, tb):
    fw = k.fw
    A = k.A
    S = A["S"]
    C = k.C
    sm = A["sm"]
    dr = k.dram
    YT = A["YT"][0]
    NCH = TBA // 64
    ident = C["ident"]
    halo = A["rw_halo"]
    colp = A["colp"]
    Lo = lambda t: t[:, 0:TBA]
    Hi = lambda t: t[:, 256:256 + TBA]
    T0 = tb * TBA

    def lerp(p, dst, dT, zt, ci, nparts=128):
        P_ = slice(0, nparts)
        k.cp("dve", zt[P_, 0:1], halo[P_, ci:ci + 1], [halo.b()], [zt.b()])
        k.cp("act", zt[P_, 1:1 + TBA], p[P_, 0:TBA], [p.b()], [zt.b()])
        k.cp("dve", halo[P_, ci:ci + 1], zt[P_, TBA:TBA + 1], [zt.b()], [halo.b()])
        k.tt("dve", dst, zt[P_, 0:TBA], zt[P_, 1:1 + TBA], ALU.subtract, [zt.b()], [dT.b()])
        k.stt(dst, dst, cp_(k, "mu", ci), zt[P_, 1:1 + TBA], ALU.mult, ALU.add, [dT.b(), colp.b(), zt.b()], [dT.b()])

    pieces = [w_in_ap(k, layer, 1536, 256)]
    if layer > 0:
        pieces.append(w_in_ap(k, layer, N_COLS_BASE, 32))
    wsv, wsb = load_w_multi(k, pieces, 8)
    ztmp = S[5]
    Z12, SG = S[0], S[0]
    p = zt_mm(k, wsv, wsb, 0, 128, tb)
    lerp(p, Lo(Z12), Z12, ztmp, 12)
    k.act(Z12[0:64, 0:TBA], Z12[0:64, 0:TBA], AF.Tanh, [Z12.b()], [Z12.b()])
    p = zt_mm(k, wsv, wsb, 128, 128, tb)
    lerp(p, Hi(SG), SG, ztmp, 13)
    k.act(Hi(SG), Hi(SG), AF.Sigmoid, [SG.b()], [SG.b()])
    ZX = S[1]
    if layer > 0:
        p = zt_mm(k, wsv, wsb, 256, 32, tb)
        k.cp("act", ZX[0:32, 0:TBA], p[0:32, 0:TBA], [p.b()], [ZX.b()])
    wa, g2 = A["rw_wa"], A["rw_g2"]
    for j in range(4):
        js = slice(j * 128, (j + 1) * 128)
        wv, wb_ = load_w_multi(k, [w_in_ap(k, layer, q * W + j * 128, 128) for q in range(3)], 8)
        zt, R, Kp, V, LP, AS, KK, E1, E2, E3, TMP = S[5], S[6], S[7], S[8], S[9], S[10], S[11], S[12], S[13], S[14], S[15]
        AB, KR, BG = S[2], S[3], S[4]
        AT_, BT_, KT_, RT_, BON, GG = Lo(AB), Hi(AB), Lo(KR), Hi(KR), Lo(BG), Hi(BG)
        KM = Hi(S[1])
        p = zt_mm(k, wv, wb_, 0, 128, tb)
        lerp(p, Lo(R), R, zt, j)
        p = zt_mm(k, wv, wb_, 128, 128, tb)
        lerp(p, Lo(Kp), Kp, zt, 4 + j)
        p = zt_mm(k, wv, wb_, 256, 128, tb)
        lerp(p, Lo(V), V, zt, 8 + j)
        pw = k.ps()
        k.mm(pw[:, 0:TBA], wa[0:64, js], Z12[0:64, 0:TBA], True, True, [wa.b(), Z12.b()], [pw.b()])
        k.act(Lo(LP), pw[:, 0:TBA], AF.Sigmoid, [pw.b(), colp.b()], [LP.b()], bias=cp_(k, "w0", j))
        k.ts("dve", Lo(LP), Lo(LP), -0.6065306597126334, None, ALU.mult, None, [LP.b()], [LP.b()])
        pa = k.ps()
        k.mm(pa[:, 0:TBA], wa[64:128, js], Z12[64:128, 0:TBA], True, True, [wa.b(), Z12.b()], [pa.b()])
        k.act(Lo(AS), pa[:, 0:TBA], AF.Sigmoid, [pa.b(), colp.b()], [AS.b()], bias=cp_(k, "a0", j))
        pg = k.ps()
        k.mm(pg[:, 0:TBA], g2[:, js], Hi(SG), True, True, [g2.b(), SG.b()], [pg.b()])
        k.cp("act", GG, pg[:, 0:TBA], [pg.b()], [BG.b()])
        vf_ap = k.vf[j * 128:(j + 1) * 128, T0:T0 + TBA]
        if layer == 0:
            if not k.cfg.get("novf"):
                fw.dma("sp", vf_ap, Lo(V), reads=[V.b()], writes=[k.vfb])
        else:
            pv = k.ps()
            k.mm(pv[:, 0:TBA], A["rw_v2"][:, js], ZX[0:32, 0:TBA], True, True, [A["rw_v2"].b(), ZX.b()], [pv.b()])
            k.act(Lo(TMP), pv[:, 0:TBA], AF.Sigmoid, [pv.b(), colp.b()], [TMP.b()], bias=cp_(k, "v0", j))
            fw.dma("sp", Lo(E1), vf_ap, reads=[k.vfb], writes=[E1.b()])
            k.tt("dve", Lo(E1), Lo(E1), Lo(V), ALU.subtract, [E1.b(), V.b()], [E1.b()])
            k.tt("dve", Lo(E1), Lo(E1), Lo(TMP), ALU.mult, [E1.b(), TMP.b()], [E1.b()])
            k.tt("dve", Lo(V), Lo(V), Lo(E1), ALU.add, [V.b(), E1.b()], [V.b()])
        k.ts("dve", Lo(KK), Lo(Kp), cp_(k, "k_k", j), None, ALU.mult, None, [Kp.b(), colp.b()], [KK.b()])
        k.tt("dve", Lo(TMP), Lo(KK), Lo(KK), ALU.mult, [KK.b()], [TMP.b()])
        pn = k.ps()
        k.mm(pn[:, 0:TBA], C["bd64"][:], Lo(TMP), True, True, [C["bd64"].b(), TMP.b()], [pn.b()])
        k.act(Lo(TMP), pn[:, 0:TBA], AF.Sqrt, [pn.b()], [TMP.b()])
        k.ts("dve", Lo(TMP), Lo(TMP), 1e-12, None, ALU.max, None, [TMP.b()], [TMP.b()])
        fw.op("dve", lambda e, TMP=TMP: e.reciprocal(out=Lo(TMP), in_=Lo(TMP)), [TMP.b()], [TMP.b()])
        k.tt("dve", Lo(KK), Lo(KK), Lo(TMP), ALU.mult, [KK.b(), TMP.b()], [KK.b()])
        k.ts("dve", KM, Lo(AS), cp_(k, "k_a", j), A["rw_c"][:, j:j + 1], ALU.mult, ALU.add, [AS.b(), colp.b(), A["rw_c"].b()], [S[1].b()])
        k.tt("dve", KM, KM, Lo(Kp), ALU.mult, [S[1].b(), Kp.b()], [S[1].b()])
        k.stt(Lo(TMP), Lo(R), cp_(k, "r_k", j), KM, ALU.mult, ALU.mult, [R.b(), colp.b(), S[1].b()], [TMP.b()])
        pb = k.ps()
        k.mm(pb[:, 0:TBA], C["bd64"][:], Lo(TMP), True, True, [C["bd64"].b(), TMP.b()], [pb.b()])
        k.tt("dve", BON, pb[:, 0:TBA], Lo(V), ALU.mult, [pb.b(), V.b()], [BG.b()])
        k.cp("dve", Lo(E3), Lo(LP), [LP.b()], [E3.b()])
        fw.op("dve", lambda e, LP=LP: e.tensor_tensor_scan(out=Lo(LP), data0=C["rmask"][:, 0:TBA], data1=Lo(LP), initial=0.0,
                                                            op0=ALU.mult, op1=ALU.add), [LP.b(), C["rmask"].b()], [LP.b()])
        k.tt("dve", Lo(E3), Lo(LP), Lo(E3), ALU.subtract, [LP.b(), E3.b()], [E3.b()])
        k.act(Lo(E3), Lo(E3), AF.Exp, [E3.b()], [E3.b()])
        k.act(Lo(E1), Lo(LP), AF.Exp, [LP.b()], [E1.b()])
        k.act(Lo(E2), Lo(LP), AF.Exp, [LP.b()], [E2.b()], scale=-1.0)
        pc = sm[:, 56:56 + NCH]
        k.cp("dve", pc, v3(Lo(E1), NCH)[:, :, 63], [E1.b()], [sm.b()])
        k.stt(AT_, Lo(KK), -1.0, Lo(E3), ALU.mult, ALU.mult, [KK.b(), E3.b()], [AB.b()])
        k.tt("dve", BT_, Lo(KK), Lo(AS), ALU.mult, [KK.b(), AS.b()], [AB.b()])
        k.tt("dve", BT_, BT_, Lo(E2), ALU.mult, [AB.b(), E2.b()], [AB.b()])
        k.tt("dve", KT_, KM, Lo(E2), ALU.mult, [S[1].b(), E2.b()], [KR.b()])
        k.tt("dve", RT_, Lo(R), Lo(E1), ALU.mult, [R.b(), E1.b()], [KR.b()])
        if k.cfg.get("rwstop") == "prep":
            continue
        Bt, Kt, Vt = S[5], S[6], S[7]
        for src_ap, srcb, dst in ((BT_, AB, Bt), (KT_, KR, Kt), (Lo(V), V, Vt)):
            p = k.ps()
            for c in range(NCH):
                k.tr(p[0:64, c * 128:(c + 1) * 128], src_ap[:, c * 64:(c + 1) * 64], ident[:], [srcb.b(), ident.b()], [p.b()])
            k.cp("act", dst[0:64, 0:NCH * 128], p[0:64, 0:NCH * 128], [p.b()], [dst.b()])
        if k.cfg.get("rwstop") == "tok":
            continue
        AK, RB, RK, Nm, NTm, Xm = S[8], S[9], S[10], S[11], S[12], S[13]
        NI = NCH * 2

        Mt = [S[13], S[14], S[15]]
        for mi, (src_ap, srcb) in enumerate(((AT_, AB), (BT_, AB), (RT_, KR))):
            for hh in range(2):
                dst_ap = Lo(Mt[mi]) if hh == 0 else Hi(Mt[mi])
                k.ts("dve", dst_ap, src_ap, C["bd64"][:, hh * 64:hh * 64 + 1], None, ALU.mult, None, [srcb.b(), C["bd64"].b()], [Mt[mi].b()])
        ATm = (Lo(Mt[0]), Hi(Mt[0]))
        BTm = (Lo(Mt[1]), Hi(Mt[1]))
        RTm = (Lo(Mt[2]), Hi(Mt[2]))

        def amat(lhs_ap, lb, rhs_m, rb, dst, mask):
            p = k.ps()
            for c in range(NCH):
                for hh in range(2):
                    i = c * 2 + hh
                    cs = slice(c * 64, (c + 1) * 64)
                    k.mm(p[0:64, i * 64:(i + 1) * 64], lhs_ap[:, cs], rhs_m[hh][:, cs], True, True, [lb.b(), rb.b()], [p.b()])
            k.tt("dve", dst[0:64, 0:NI * 64], p[0:64, 0:NI * 64], C[mask][0:64, 0:NI * 64], ALU.mult, [p.b(), C[mask].b()], [dst.b()])
        amat(BT_, AB, ATm, Mt[0], Nm, "m_strict")
        amat(AT_, AB, BTm, Mt[1], NTm, "m_lows")
        amat(KT_, KR, ATm, Mt[0], AK, "m_strict")
        amat(BT_, AB, RTm, Mt[2], RB, "m_incl")
        amat(KT_, KR, RTm, Mt[2], RK, "m_incl")
        if k.cfg.get("rwstop") == "amat":
            continue
        idb = ident[0:64, 0:64].unsqueeze(1).broadcast_to([64, NI, 64])
        k.tt("dve", v3(Xm[0:64, 0:NI * 64], NI), v3(Nm[0:64, 0:NI * 64], NI), idb, ALU.add, [Nm.b(), ident.b()], [Xm.b()])
        for lvl in range(5):
            pP = k.ps() if lvl < 4 else None
            pPT = k.ps()
            for i in range(NI):
                cs = slice(i * 64, (i + 1) * 64)
                if pP is not None:
                    k.mm(pP[0:64, cs], NTm[0:64, cs], Nm[0:64, cs], True, True, [NTm.b(), Nm.b()], [pP.b()])
                k.mm(pPT[0:64, cs], Nm[0:64, cs], NTm[0:64, cs], True, True, [NTm.b(), Nm.b()], [pPT.b()])
            if pP is not None:
                k.cp("act", Nm[0:64, 0:NI * 64], pP[0:64, 0:NI * 64], [pP.b()], [Nm.b()])
            k.cp("dve", NTm[0:64, 0:NI * 64], pPT[0:64, 0:NI * 64], [pPT.b()], [NTm.b()])
            pX = k.ps()
            for i in range(NI):
                cs = slice(i * 64, (i + 1) * 64)
                k.mm(pX[0:64, cs], NTm[0:64, cs], Xm[0:64, cs], True, True, [NTm.b(), Xm.b()], [pX.b()])
            k.tt("dve", Xm[0:64, 0:NI * 64], Xm[0:64, 0:NI * 64], pX[0:64, 0:NI * 64], ALU.add, [Xm.b(), pX.b()], [Xm.b()])
        if k.cfg.get("rwstop") == "inv":
            continue
        ST = A["rw_ST"][j]
        Yb, Ws = S[14], S[15]
        for c in range(NCH):
            cs = slice(c * 64, (c + 1) * 64)
            tok = slice(c * 128, (c + 1) * 128)
            pw0 = k.ps()
            k.mm(pw0[0:64, 0:128], AT_[:, cs], ST[:], True, False, [AB.b(), ST.b()], [pw0.b()])
            for hh in range(2):
                i = c * 2 + hh
                hv = slice(hh * 64, (hh + 1) * 64)
                k.mm(pw0[0:64, hv], AK[0:64, i * 64:(i + 1) * 64], Vt[0:64, c * 128 + hh * 64:c * 128 + (hh + 1) * 64], False, hh == 1,
                     [AK.b(), Vt.b()], [pw0.b()])
            k.cp("act", Ws[0:64, 0:128], pw0[0:64, 0:128], [pw0.b()], [Ws.b()])
            pu = k.ps()
            for hh in range(2):
                i = c * 2 + hh
                hv = slice(hh * 64, (hh + 1) * 64)
                k.mm(pu[0:64, hv], Xm[0:64, i * 64:(i + 1) * 64], Ws[0:64, hv], True, True, [Xm.b(), Ws.b()], [pu.b()])
            k.cp("act", Ws[0:64, 128:256], pu[0:64, 0:128], [pu.b()], [Ws.b()])
            Us = Ws[0:64, 128:256]
            py = k.ps()
            k.mm(py[0:64, 0:128], RT_[:, cs], ST[:], True, False, [KR.b(), ST.b()], [py.b()])
            for hh in range(2):
                i = c * 2 + hh
                hv = slice(hh * 64, (hh + 1) * 64)
                k.mm(py[0:64, hv], RB[0:64, i * 64:(i + 1) * 64], Ws[0:64, 128 + hh * 64:128 + (hh + 1) * 64], False, False,
                     [RB.b(), Ws.b()], [py.b()])
                k.mm(py[0:64, hv], RK[0:64, i * 64:(i + 1) * 64], Vt[0:64, c * 128 + hh * 64:c * 128 + (hh + 1) * 64], False, hh == 1,
                     [RK.b(), Vt.b()], [py.b()])
            k.cp("act", Yb[0:64, tok], py[0:64, 0:128], [py.b()], [Yb.b()])
            pS = k.ps()
            k.mm(pS[:, 0:128], Bt[0:64, tok], Us, True, False, [Bt.b(), Ws.b()], [pS.b()])
            k.mm(pS[:, 0:128], Kt[0:64, tok], Vt[0:64, tok], False, True, [Kt.b(), Vt.b()], [pS.b()])
            for hh in range(2):
                P_ = slice(hh * 64, (hh + 1) * 64)
                k.ts("dve", ST[P_, P_], ST[P_, P_], sm[P_, 56 + c:57 + c], None, ALU.mult, None, [ST.b(), sm.b()], [ST.b()])
                k.stt(ST[P_, P_], pS[P_, P_], sm[P_, 56 + c:57 + c], ST[P_, P_], ALU.mult, ALU.add, [pS.b(), sm.b(), ST.b()], [ST.b()])
        if k.cfg.get("rwstop") == "seq":
            continue
        NG = NCH * 2
        Y3 = v3(Yb[0:64, 0:NG * 64], NG)
        sqt = S[8]
        sq3 = v3(sqt[0:64, 0:NG * 64], NG)
        s1, s2, mean, rstd = sm[0:64, 0:8], sm[0:64, 8:16], sm[0:64, 16:24], sm[0:64, 24:32]
        bsc = lambda a: v3(a, NG).broadcast_to([64, NG, 64])
        fw.op("dve", lambda e, Y3=Y3, s1=s1: e.tensor_reduce(out=s1, in_=Y3, axis=AX.X, op=ALU.add), [Yb.b()], [sm.b()])
        k.tt("dve", sq3, Y3, Y3, ALU.mult, [Yb.b()], [sqt.b()])
        fw.op("dve", lambda e, sq3=sq3, s2=s2: e.tensor_reduce(out=s2, in_=sq3, axis=AX.X, op=ALU.add), [sqt.b()], [sm.b()])
        k.ts("dve", mean, s1, 1.0 / 64, None, ALU.mult, None, [sm.b()], [sm.b()])
        k.tt("dve", s1, mean, mean, ALU.mult, [sm.b()], [sm.b()])
        k.ts("dve", s2, s2, 1.0 / 64, 64e-5, ALU.mult, ALU.add, [sm.b()], [sm.b()])
        k.tt("dve", s2, s2, s1, ALU.subtract, [sm.b()], [sm.b()])
        k.act(s2, s2, AF.Sqrt, [sm.b()], [sm.b()])
        fw.op("dve", lambda e, s2=s2, rstd=rstd: e.reciprocal(out=rstd, in_=s2), [sm.b()], [sm.b()])
        k.tt("dve", Y3, Y3, bsc(mean), ALU.subtract, [Yb.b(), sm.b()], [Yb.b()])
        k.tt("dve", Y3, Y3, bsc(rstd), ALU.mult, [Yb.b(), sm.b()], [Yb.b()])
        pT = k.ps()
        for c in range(NCH):
            k.tr(pT[:, c * 64:(c + 1) * 64], Yb[0:64, c * 128:(c + 1) * 128], ident[0:64, 0:64], [Yb.b(), ident.b()], [pT.b()])
        k.ts("dve", Lo(TMP), pT[:, 0:TBA], cp_(k, "lnx_g", j), cp_(k, "lnx_b", j), ALU.mult, ALU.add, [pT.b(), colp.b()], [TMP.b()])
        k.tt("dve", Lo(TMP), Lo(TMP), BON, ALU.add, [TMP.b(), BG.b()], [TMP.b()])
        k.tt("dve", YT[:, j, :], Lo(TMP), GG, ALU.mult, [TMP.b(), BG.b()], [YT.b()])
```
